# Optimizing a Trainium2 kernel written in Bass

```python
import math
import jax, jax.numpy as jnp
from jax import lax
import numpy as np

D_MODEL = 1024
BATCH = 2
SEQ = 16384
DEPTH = 2
DEC_BATCH = 2
DEC_SEQ = 8192
PAST_LEN = 128

HEAD_DIM = 64
DIFF_HEADS = 4
DIFF_DH = 64
SWA_HEADS = 8
SWA_KV_HEADS = 2
SWA_WINDOW = 128
SWA_BLOCK = 128
MLA_HEADS = 8
MLA_Q_RANK = 256
MLA_KV_RANK = 128
MLA_NOPE = 64
MLA_ROPE = 32
MLA_VDIM = 64
NA_HEADS = 8
NA_DH = 64
NA_KR_MAX = 8
NA_KC = 16
NA_QB = 16
NA_KB = 32
GRID_W = 64
N_EXPERTS = 16
EC_CAPACITY_FACTOR = 2
D_EXPERT = 2816

ROPE_THETA = 10000.0
Q_BLOCK = 128
DEEPNORM_ALPHA = (2 * DEPTH) ** 0.25
DEEPNORM_BETA = (8 * DEPTH) ** -0.25
N_EVEN = (DEPTH + 1) // 2
N_ODD = DEPTH // 2
LN_EPS = 1e-5
RMS_EPS = 1e-6
NEG_INF = -1e30

EVEN_SPLIT = (DIFF_HEADS * 2 * DIFF_DH, DIFF_HEADS * 2 * DIFF_DH, DIFF_HEADS * 2 * DIFF_DH,
              SWA_HEADS * HEAD_DIM, SWA_KV_HEADS * HEAD_DIM, SWA_KV_HEADS * HEAD_DIM)
EVEN_IN = sum(EVEN_SPLIT)
EVEN_OUT = DIFF_HEADS * 2 * DIFF_DH + SWA_HEADS * HEAD_DIM
ODD_SPLIT = (MLA_Q_RANK, MLA_KV_RANK, MLA_ROPE,
             NA_HEADS * NA_DH, NA_HEADS * NA_DH, NA_HEADS * NA_DH)
ODD_IN = sum(ODD_SPLIT)
ODD_OUT = MLA_HEADS * MLA_VDIM + NA_HEADS * NA_DH

kernel_name = 'hybrid_diff_swa_mla_na_ec_encoder'


def split_cols(x, sizes):
    outs, off = [], 0
    for n in sizes:
        outs.append(x[..., off:off + n])
        off += n
    return outs


def layer_norm(x, g, b):
    xf = x.astype(jnp.float32)
    mu = jnp.mean(xf, -1, keepdims=True)
    var = jnp.mean(jnp.square(xf - mu), -1, keepdims=True)
    y = (xf - mu) * lax.rsqrt(var + LN_EPS) * g.astype(jnp.float32) + b.astype(jnp.float32)
    return y.astype(x.dtype)


def rms_norm(x, g):
    xf = x.astype(jnp.float32)
    y = xf * lax.rsqrt(jnp.mean(jnp.square(xf), -1, keepdims=True) + RMS_EPS) * g.astype(jnp.float32)
    return y.astype(x.dtype)


def rope_tables(seq, dim):
    inv = 1.0 / (ROPE_THETA ** (jnp.arange(0, dim, 2, dtype=jnp.float32) / dim))
    ang = jnp.arange(seq, dtype=jnp.float32)[:, None] * inv[None, :]
    return jnp.cos(ang), jnp.sin(ang)


def apply_rope(x, cos, sin):
    half = x.shape[-1] // 2
    x1 = x[..., :half].astype(jnp.float32)
    x2 = x[..., half:].astype(jnp.float32)
    c = cos[None, :, None, :]
    s = sin[None, :, None, :]
    return jnp.concatenate([x1 * c - x2 * s, x2 * c + x1 * s], -1).astype(x.dtype)


def diff_attention(q, k, v, lam, subln, lam_init):
    B, S, H, _, dh = q.shape
    nb = S // Q_BLOCK
    scale = dh ** -0.5
    lf = lam.astype(jnp.float32)
    lmbda = jnp.exp(jnp.sum(lf[0] * lf[1])) - jnp.exp(jnp.sum(lf[2] * lf[3])) + lam_init
    qb = q.reshape(B, nb, Q_BLOCK, H, 2, dh).swapaxes(0, 1)

    def block(qblk):
        s = jnp.einsum('bqhmd,bkhmd->bhmqk', qblk, k).astype(jnp.float32) * scale
        p = jax.nn.softmax(s, axis=-1)
        a = p[:, :, 0] - lmbda * p[:, :, 1]
        return jnp.einsum('bhqk,bkhd->bqhd', a.astype(v.dtype), v)

    o = lax.map(block, qb).swapaxes(0, 1).reshape(B, S, H, 2 * dh)
    o = rms_norm(o, subln) * (1.0 - lam_init)
    return o.reshape(B, S, H * 2 * dh)


def window_gqa(q, k, v, sink):
    B, S, H, dh = q.shape
    G = k.shape[2]
    R = H // G
    W = SWA_BLOCK
    nb = S // W
    pad = ((0, 0), (W, W), (0, 0), (0, 0))
    kp = jnp.pad(k, pad).reshape(B, nb + 2, W, G, dh)
    vp = jnp.pad(v, pad).reshape(B, nb + 2, W, G, dh)
    kw = jnp.concatenate([kp[:, :-2], kp[:, 1:-1], kp[:, 2:]], axis=2)
    vw = jnp.concatenate([vp[:, :-2], vp[:, 1:-1], vp[:, 2:]], axis=2)
    qb = q.reshape(B, nb, W, G, R, dh)
    s = jnp.einsum('bnqgrd,bnkgd->bngrqk', qb, kw).astype(jnp.float32) * dh ** -0.5
    blk = jnp.arange(nb)[:, None] * W
    qpos = blk + jnp.arange(W)[None, :]
    kpos = blk - W + jnp.arange(3 * W)[None, :]
    valid = ((kpos[:, None, :] >= 0) & (kpos[:, None, :] < S)
             & (jnp.abs(qpos[:, :, None] - kpos[:, None, :]) <= SWA_WINDOW))
    s = jnp.where(valid[None, :, None, None], s, NEG_INF)
    sink_l = sink.astype(jnp.float32).reshape(G, R)[None, None, :, :, None, None]
    m = jnp.maximum(jnp.max(s, -1, keepdims=True), sink_l)
    e = jnp.exp(s - m)
    p = e / (jnp.sum(e, -1, keepdims=True) + jnp.exp(sink_l - m))
    o = jnp.einsum('bngrqk,bnkgd->bnqgrd', p.astype(v.dtype), vw)
    return o.reshape(B, S, H * dh)


def mla_attention(c_q, c_kv, k_rope, q_norm, w_uq, kv_norm, w_ukv, cos, sin):
    B, S, _ = c_q.shape
    H = MLA_HEADS
    q = (rms_norm(c_q, q_norm) @ w_uq).reshape(B, S, H, MLA_NOPE + MLA_ROPE)
    q_nope, q_pe = q[..., :MLA_NOPE], apply_rope(q[..., MLA_NOPE:], cos, sin)
    kv = (rms_norm(c_kv, kv_norm) @ w_ukv).reshape(B, S, H, MLA_NOPE + MLA_VDIM)
    k_nope, v = kv[..., :MLA_NOPE], kv[..., MLA_NOPE:]
    k_pe = apply_rope(k_rope[:, :, None, :], cos, sin)[:, :, 0]
    scale = (MLA_NOPE + MLA_ROPE) ** -0.5
    nb = S // Q_BLOCK
    qn_b = q_nope.reshape(B, nb, Q_BLOCK, H, MLA_NOPE).swapaxes(0, 1)
    qp_b = q_pe.reshape(B, nb, Q_BLOCK, H, MLA_ROPE).swapaxes(0, 1)

    def block(args):
        qn, qp = args
        s = (jnp.einsum('bqhd,bkhd->bhqk', qn, k_nope)
             + jnp.einsum('bqhd,bkd->bhqk', qp, k_pe)).astype(jnp.float32) * scale
        p = jax.nn.softmax(s, axis=-1)
        return jnp.einsum('bhqk,bkhd->bqhd', p.astype(v.dtype), v)

    o = lax.map(block, (qn_b, qp_b)).swapaxes(0, 1)
    return o.reshape(B, S, H * MLA_VDIM)


def neighbourhood_attention(q, k, v, rpb):
    B, S, H, dh = q.shape
    rows = S // GRID_W
    kr = min(NA_KR_MAX, rows)
    nj = GRID_W // NA_QB
    qg = q.reshape(B, rows, nj, NA_QB, H, dh)
    kg = k.reshape(B, rows, GRID_W, H, dh)
    vg = v.reshape(B, rows, GRID_W, H, dh)
    r = jnp.arange(rows)
    row_idx = jnp.clip(r - kr // 2, 0, rows - kr)[:, None] + jnp.arange(kr)[None, :]
    j = jnp.arange(nj)
    col_idx = jnp.clip(j * NA_QB - NA_KC // 2, 0, GRID_W - NA_KB)[:, None] + jnp.arange(NA_KB)[None, :]
    ri = row_idx[:, None, :, None]
    ci = col_idx[None, :, None, :]
    kw = kg[:, ri, ci]
    vw = vg[:, ri, ci]
    s = jnp.einsum('brjqhd,brjkchd->brjhqkc', qg, kw).astype(jnp.float32) * dh ** -0.5
    qcol = j[:, None] * NA_QB + jnp.arange(NA_QB)[None, :]
    qcs = jnp.clip(qcol - NA_KC // 2, 0, GRID_W - NA_KC)
    kcol = col_idx[:, None, :]
    valid = (kcol >= qcs[:, :, None]) & (kcol < qcs[:, :, None] + NA_KC)
    dr = row_idx - r[:, None] + (NA_KR_MAX - 1)
    dc = jnp.clip(kcol - qcol[:, :, None] + (NA_KC - 1), 0, 2 * NA_KC - 2)
    bias = rpb[:, dr[:, None, None, :, None], dc[None, :, :, None, :]]
    s = s + bias.transpose(1, 2, 0, 3, 4, 5)[None].astype(jnp.float32)
    s = jnp.where(valid[None, None, :, None, :, None, :], s, NEG_INF)
    p = jax.nn.softmax(s.reshape(s.shape[:5] + (kr * NA_KB,)), axis=-1).reshape(s.shape)
    o = jnp.einsum('brjhqkc,brjkchd->brjqhd', p.astype(v.dtype), vw)
    return o.reshape(B, S, H * dh)


def even_mixer(h, w_in, lam, subln, sink, w_out, cos, sin, lam_init):
    B, S, _ = h.shape
    qa, ka, va, qb, kb, vb = split_cols(h @ w_in, EVEN_SPLIT)
    qa = apply_rope(qa.reshape(B, S, DIFF_HEADS * 2, DIFF_DH), cos, sin).reshape(B, S, DIFF_HEADS, 2, DIFF_DH)
    ka = apply_rope(ka.reshape(B, S, DIFF_HEADS * 2, DIFF_DH), cos, sin).reshape(B, S, DIFF_HEADS, 2, DIFF_DH)
    va = va.reshape(B, S, DIFF_HEADS, 2 * DIFF_DH)
    oa = diff_attention(qa, ka, va, lam, subln, lam_init)
    qb = apply_rope(qb.reshape(B, S, SWA_HEADS, HEAD_DIM), cos, sin)
    kb = apply_rope(kb.reshape(B, S, SWA_KV_HEADS, HEAD_DIM), cos, sin)
    vb = vb.reshape(B, S, SWA_KV_HEADS, HEAD_DIM)
    ob = window_gqa(qb, kb, vb, sink)
    return jnp.concatenate([oa, ob], -1) @ w_out


def odd_mixer(h, w_in, q_norm, w_uq, kv_norm, w_ukv, rpb, w_out, cos, sin):
    B, S, _ = h.shape
    c_q, c_kv, k_rope, qn, kn, vn = split_cols(h @ w_in, ODD_SPLIT)
    oc = mla_attention(c_q, c_kv, k_rope, q_norm, w_uq, kv_norm, w_ukv, cos, sin)
    shp = (B, S, NA_HEADS, NA_DH)
    od = neighbourhood_attention(qn.reshape(shp), kn.reshape(shp), vn.reshape(shp), rpb)
    return jnp.concatenate([oc, od], -1) @ w_out


def expert_choice_ffn(h, router_w, w_gate, w_up, w_down):
    B, S, D = h.shape
    n = B * S
    cap = EC_CAPACITY_FACTOR * n // N_EXPERTS
    xf = h.reshape(n, D)
    aff = jax.nn.softmax((xf @ router_w).astype(jnp.float32), axis=-1)
    gates, idx = lax.top_k(aff.T, cap)
    xe = xf[idx]
    a = jnp.einsum('ecd,edf->ecf', xe, w_gate)
    u = jnp.einsum('ecd,edf->ecf', xe, w_up)
    y = jnp.einsum('ecf,efd->ecd', jax.nn.silu(a) * u, w_down) * gates[..., None].astype(h.dtype)
    out = jnp.zeros_like(xf).at[idx.reshape(-1)].add(y.reshape(-1, D))
    return out.reshape(B, S, D)


def trunk(x, c, w_in_even, diff_lambda, diff_subln, swa_sink, w_out_even,
          w_in_odd, mla_q_norm, mla_w_uq, mla_kv_norm, mla_w_ukv, na_rpb, w_out_odd,
          ada_w, ada_b, ln_g, ln_b, router_w, exp_w_gate, exp_w_up, exp_w_down):
    S = x.shape[1]
    cos_h, sin_h = rope_tables(S, HEAD_DIM)
    cos_r, sin_r = rope_tables(S, MLA_ROPE)
    c_act = jax.nn.silu(c)
    for l in range(DEPTH):
        mod = (c_act @ ada_w[l] + ada_b[l])[:, None, :]
        sh1, sc1, g1, sh2, sc2, g2 = split_cols(mod, (D_MODEL,) * 6)
        h = x * (1 + sc1) + sh1
        if l % 2 == 0:
            e = l // 2
            lam_init = 0.8 - 0.6 * math.exp(-0.3 * l)
            y = even_mixer(h, w_in_even[e], diff_lambda[e], diff_subln[e], swa_sink[e], w_out_even[e],
                           cos_h, sin_h, lam_init)
        else:
            o = l // 2
            y = odd_mixer(h, w_in_odd[o], mla_q_norm[o], mla_w_uq[o], mla_kv_norm[o], mla_w_ukv[o],
                          na_rpb[o], w_out_odd[o], cos_r, sin_r)
        x = layer_norm(DEEPNORM_ALPHA * x + g1 * y, ln_g[l, 0], ln_b[l, 0])
        h = x * (1 + sc2) + sh2
        y = expert_choice_ffn(h, router_w[l], exp_w_gate[l], exp_w_up[l], exp_w_down[l])
        x = layer_norm(DEEPNORM_ALPHA * x + g2 * y, ln_g[l, 1], ln_b[l, 1])
    return x


def setup_inputs(seed: int = 0) -> dict:
    key = jax.random.key(seed)
    ks = jax.random.split(key, 24)
    D = D_MODEL
    nrm = lambda k, shp, sc: jax.random.normal(k, shp, jnp.float32) * sc
    return {
        'x_prompt': nrm(ks[0], (BATCH, SEQ, D), 1.0),
        'x_sample': nrm(ks[1], (DEC_BATCH, DEC_SEQ, D), 1.0),
        'c_prompt': nrm(ks[2], (BATCH, D), 1.0),
        'c_sample': nrm(ks[3], (DEC_BATCH, D), 1.0),
        'w_in_even': nrm(ks[4], (N_EVEN, D, EVEN_IN), D ** -0.5),
        'diff_lambda': nrm(ks[5], (N_EVEN, 4, DIFF_DH), 0.1),
        'diff_subln': 1.0 + nrm(ks[6], (N_EVEN, 2 * DIFF_DH), 0.02),
        'swa_sink': nrm(ks[7], (N_EVEN, SWA_HEADS), 0.5),
        'w_out_even': nrm(ks[8], (N_EVEN, EVEN_OUT, D), EVEN_OUT ** -0.5 * DEEPNORM_BETA),
        'w_in_odd': nrm(ks[9], (N_ODD, D, ODD_IN), D ** -0.5),
        'mla_q_norm': 1.0 + nrm(ks[10], (N_ODD, MLA_Q_RANK), 0.02),
        'mla_w_uq': nrm(ks[11], (N_ODD, MLA_Q_RANK, MLA_HEADS * (MLA_NOPE + MLA_ROPE)), MLA_Q_RANK ** -0.5),
        'mla_kv_norm': 1.0 + nrm(ks[12], (N_ODD, MLA_KV_RANK), 0.02),
        'mla_w_ukv': nrm(ks[13], (N_ODD, MLA_KV_RANK, MLA_HEADS * (MLA_NOPE + MLA_VDIM)), MLA_KV_RANK ** -0.5),
        'na_rpb': nrm(ks[14], (N_ODD, NA_HEADS, 2 * NA_KR_MAX - 1, 2 * NA_KC - 1), 0.1),
        'w_out_odd': nrm(ks[15], (N_ODD, ODD_OUT, D), ODD_OUT ** -0.5 * DEEPNORM_BETA),
        'ada_w': nrm(ks[16], (DEPTH, D, 6 * D), D ** -0.5),
        'ada_b': nrm(ks[17], (DEPTH, 6 * D), 0.02),
        'ln_g': 1.0 + nrm(ks[18], (DEPTH, 2, D), 0.02),
        'ln_b': nrm(ks[19], (DEPTH, 2, D), 0.02),
        'router_w': nrm(ks[20], (DEPTH, D, N_EXPERTS), D ** -0.5),
        'exp_w_gate': nrm(ks[21], (DEPTH, N_EXPERTS, D, D_EXPERT), D ** -0.5),
        'exp_w_up': nrm(ks[22], (DEPTH, N_EXPERTS, D, D_EXPERT), D ** -0.5),
        'exp_w_down': nrm(ks[23], (DEPTH, N_EXPERTS, D_EXPERT, D), D_EXPERT ** -0.5 * DEEPNORM_BETA),
    }


def reference(x_prompt, x_sample, c_prompt, c_sample, w_in_even, diff_lambda, diff_subln, swa_sink,
              w_out_even, w_in_odd, mla_q_norm, mla_w_uq, mla_kv_norm, mla_w_ukv, na_rpb, w_out_odd,
              ada_w, ada_b, ln_g, ln_b, router_w, exp_w_gate, exp_w_up, exp_w_down):
    y_prompt = trunk(x_prompt, c_prompt, w_in_even, diff_lambda, diff_subln, swa_sink, w_out_even,
                     w_in_odd, mla_q_norm, mla_w_uq, mla_kv_norm, mla_w_ukv, na_rpb, w_out_odd,
                     ada_w, ada_b, ln_g, ln_b, router_w, exp_w_gate, exp_w_up, exp_w_down)
    y_sample = trunk(x_sample, c_sample, w_in_even, diff_lambda, diff_subln, swa_sink, w_out_even,
                     w_in_odd, mla_q_norm, mla_w_uq, mla_kv_norm, mla_w_ukv, na_rpb, w_out_odd,
                     ada_w, ada_b, ln_g, ln_b, router_w, exp_w_gate, exp_w_up, exp_w_down)
    return (y_prompt, y_sample)
```

```python
import numpy as np
from contextlib import ExitStack
import concourse.bass as bass
import concourse.mybir as mybir
from concourse.bass_utils import run_bass_kernel_spmd

F32 = mybir.dt.float32
BF16 = mybir.dt.bfloat16
I32 = mybir.dt.int32
AF = mybir.ActivationFunctionType
ALU = mybir.AluOpType

NCORES = 8
D = 1024
KC = D // 128
EVEN_IN = 2304


class _Op:
    __slots__ = ("eng", "fn", "deps", "signals", "dma_key", "sig", "inc", "dneed")

    def __init__(self, eng, fn, dma_key, inc):
        self.eng = eng
        self.fn = fn
        self.deps = ()
        self.dneed = None
        self.signals = dma_key is not None
        self.dma_key = dma_key
        self.sig = None
        self.inc = inc


class _Res:
    __slots__ = ("w", "r")

    def __init__(self):
        self.w = None
        self.r = []


class Prog:
    ENGS = ("pe", "act", "dve", "pool", "sp")

    def __init__(self, nc):
        self.nc = nc
        self.ops = {e: [] for e in self.ENGS}
        self.res = {}
        self.dma_cnt = {}
        self.last = {e: None for e in self.ENGS}
        self.last_dma = {}
        self.key_slot = {}
        self.free_slots = []
        self.n_slots = 0
        self.final_sig = {}

    def op(self, eng, fn, reads=(), writes=(), dma_key=None, inc=16):
        o = _Op(eng, fn, dma_key, inc)
        deps = set()
        res = self.res
        for k in reads:
            st = res.get(k)
            if st is None:
                st = res[k] = _Res()
            if st.w is not None:
                deps.add(st.w)
        for k in writes:
            st = res.get(k)
            if st is None:
                st = res[k] = _Res()
            if st.w is not None:
                deps.add(st.w)
            deps.update(st.r)
        for k in reads:
            res[k].r.append(o)
        for k in writes:
            st = res[k]
            st.w = o
            st.r = []
        deps.discard(o)
        if eng == "pe":
            deps = {d for d in deps if not (d.eng == "pe" and d.dma_key is None)}
        for d in deps:
            d.signals = True
        o.deps = tuple(deps)
        dn = None
        for d in deps:
            if d.dma_key is not None:
                if dn is None:
                    dn = {}
                sl = d.sig[0][1]
                dn[sl] = self.dma_cnt[sl]
        o.dneed = dn
        if dma_key is not None:
            slot = self.key_slot.get(dma_key)
            if slot is None:
                if self.free_slots:
                    slot = self.free_slots.pop()
                else:
                    slot = self.n_slots
                    self.n_slots += 1
                self.key_slot[dma_key] = slot
            c = self.dma_cnt.get(slot, 0) + inc
            self.dma_cnt[slot] = c
            o.sig = (("dma", slot), c)
            self.final_sig[dma_key] = (slot, c)
        self.ops[eng].append(o)
        self.last[eng] = o
        if dma_key is not None:
            self.last_dma[dma_key] = o
        return o

    def barrier(self):
        prev = [o for o in self.last.values() if o is not None] + list(self.last_dma.values())
        for e in self.ENGS:
            o = _Op(e, (lambda eng: None), None, 16)
            o.inc = -1
            deps = {d for d in prev if not (d.eng == e and d.dma_key is None)}
            for d in deps:
                d.signals = True
            o.deps = tuple(deps)
            o.dneed = {d.sig[0][1]: self.dma_cnt[d.sig[0][1]] for d in deps if d.dma_key is not None}
            self.ops[e].append(o)
        self.res = {}
        self.free_slots.extend(sl for sl in self.key_slot.values() if self.dma_cnt.get(sl, 0) < 30000)
        self.key_slot = {}
        self.last_dma = {}

    def emit(self, final_waits=()):
        nc = self.nc
        gens = {}
        for e in self.ENGS:
            c = 0
            g = 0
            for o in self.ops[e]:
                if o.inc == -1 and c > 30000:
                    g += 1
                    c = 0
                if o.dma_key is None and o.signals:
                    c += 1
                    o.sig = (("eng", e, g), c)
            gens[e] = g + 1
        with ExitStack() as es:
            sems = {}
            for e in self.ENGS:
                for g in range(gens[e]):
                    sems[("eng", e, g)] = es.enter_context(nc.semaphore("s_%s%d" % (e, g)))
            for k in range(self.n_slots):
                sems[("dma", k)] = es.enter_context(nc.semaphore("d_%d" % k))
            block = es.enter_context(nc.Block())
            prog = self

            def run(engname, eng):
                waited = {}
                for o in prog.ops[engname]:
                    need = {}
                    for d in o.deps:
                        sk, v = d.sig
                        if d.dma_key is not None:
                            v = o.dneed[sk[1]]
                        if need.get(sk, 0) < v:
                            need[sk] = v
                    for sk, v in need.items():
                        if waited.get(sk, 0) < v:
                            eng.wait_ge(sems[sk], v)
                            waited[sk] = v
                    ins = o.fn(eng)
                    if o.signals and ins is not None:
                        sk, v = o.sig
                        ins.then_inc(sems[sk], o.inc if sk[0] == "dma" else 1)
                if engname == "sp":
                    fin = {}
                    for k in final_waits:
                        if k not in prog.final_sig:
                            continue
                        slot, v = prog.final_sig[k]
                        fin[slot] = max(fin.get(slot, 0), v)
                    for slot, v in fin.items():
                        eng.wait_ge(sems[("dma", slot)], v)

            block.tensor(lambda pe: run("pe", pe))
            block.scalar(lambda act: run("act", act))
            block.vector(lambda dve: run("dve", dve))
            block.gpsimd(lambda pool: run("pool", pool))
            block.sync(lambda sp: run("sp", sp))


class Cfg:
    def __init__(self, seq_p=16384, seq_s=8192):
        self.seq_p, self.seq_s = seq_p, seq_s
        self.lp, self.ls = seq_p // NCORES, seq_s // NCORES
        self.segs = [(0, self.lp, seq_p), (self.lp, self.lp, seq_p),
                     (2 * self.lp, self.ls, seq_s), (2 * self.lp + self.ls, self.ls, seq_s)]
        self.nt = 2 * self.lp + 2 * self.ls


ALPHA = 4 ** 0.25
FH = 1408
NFC = FH // 128


def seg_of_tile(cfg, i):
    t = i * 128
    for b, (off, ln, S) in enumerate(cfg.segs):
        if off <= t < off + ln:
            return b
    raise AssertionError


def resid_ln_phase(nc, P, BK, cfg, tag, l, j, mod, ln_g, ln_b, x_src, x_dst, y_kind, y_src, w_dram, g_off, stop=False):
    NT = cfg.nt
    bk = lambda i: ("bank", i)
    with ExitStack() as ph:
        psb = lambda n, s, dt: ph.enter_context(nc.sbuf_tensor(tag + n, s, dt))
        gb = psb("gb", [128, 4, D], F32)
        lg = psb("lg", [128, D], F32)
        lb = psb("lb", [128, D], F32)
        for b in range(4):
            P.op("sp", lambda e, b=b: e.dma_start(out=gb[:, b, :], in_=mod[l, b:b + 1, g_off:g_off + D].broadcast_to([128, D])),
                 writes=[("gb", b)], dma_key=tag + "c0")
        P.op("sp", lambda e: e.dma_start(out=lg[:], in_=ln_g[l, j:j + 1, :].broadcast_to([128, D])), writes=["lg"], dma_key=tag + "c1")
        P.op("sp", lambda e: e.dma_start(out=lb[:], in_=ln_b[l, j:j + 1, :].broadcast_to([128, D])), writes=["lb"], dma_key=tag + "c2")
        if y_kind == "proj":
            wo = psb("wo", [128, KC, D], BF16)
            for k in range(KC):
                P.op("pool", lambda e, k=k: e.dma_start(out=wo[:, k, :], in_=w_dram[k * 128:(k + 1) * 128, :]),
                     writes=[("wo", k)], dma_key=tag + "wo")
            wo_keys = [("wo", k) for k in range(KC)]
            at = [psb("at%d" % i, [128, KC, 128], BF16) for i in range(2)]
        else:
            ya = [psb("ya%d" % i, [128, D], F32) for i in range(2)]
        xr = [psb("xr%d" % i, [128, D], F32) for i in range(2)]
        zz = [psb("zz%d" % i, [128, D], F32) for i in range(2)]
        st = psb("st", [128, 2, 6], F32)
        mv = psb("mv", [128, 4], F32)
        for i in range(NT // 128):
            b = seg_of_tile(cfg, i)
            s2 = i % 2
            r0 = i * 128
            P.op("sp", lambda e, s2=s2, r0=r0: e.dma_start(out=xr[s2][:], in_=x_src[r0:r0 + 128, :]),
                 writes=[("xr", s2)], dma_key=tag + "xr%d" % s2)
            if y_kind == "proj":
                P.op("sp", lambda e, s2=s2, r0=r0: e.dma_start(out=at[s2][:], in_=y_src[:, :, r0:r0 + 128].rearrange("k p t -> p k t")),
                     writes=[("at", s2)], dma_key=tag + "at%d" % s2)
                for dh in range(2):
                    pb = 2 * s2 + dh
                    for k in range(KC):
                        P.op("pe", lambda e, pb=pb, s2=s2, k=k, dh=dh: e.matmul(
                            BK[pb][:, :], lhsT=at[s2][:, k, :], rhs=wo[:, k, dh * 512:(dh + 1) * 512],
                            start=(k == 0), stop=(k == KC - 1)),
                            reads=[("at", s2)] + wo_keys, writes=[bk(pb)])
                    P.op("dve", lambda e, pb=pb, s2=s2, dh=dh, b=b: e.tensor_tensor(
                        out=zz[s2][:, dh * 512:(dh + 1) * 512], in0=BK[pb][:, :], in1=gb[:, b, dh * 512:(dh + 1) * 512], op=ALU.mult),
                        reads=[bk(pb), ("gb", b)], writes=[("zz", s2, dh)])
            else:
                P.op("sp", lambda e, s2=s2, r0=r0: e.dma_start(out=ya[s2][:], in_=y_src[r0:r0 + 128, :]),
                     writes=[("ya", s2)], dma_key=tag + "ya%d" % s2)
                for dh in range(2):
                    P.op("pool", lambda e, s2=s2, dh=dh, b=b: e.tensor_tensor(
                        out=zz[s2][:, dh * 512:(dh + 1) * 512], in0=ya[s2][:, dh * 512:(dh + 1) * 512],
                        in1=gb[:, b, dh * 512:(dh + 1) * 512], op=ALU.mult),
                        reads=[("ya", s2), ("gb", b)], writes=[("zz", s2, dh)])
            zk = [("zz", s2, 0), ("zz", s2, 1)]
            P.op("dve", lambda e, s2=s2: e.scalar_tensor_tensor(out=zz[s2][:], in0=xr[s2][:], scalar=ALPHA, in1=zz[s2][:],
                                                               op0=ALU.mult, op1=ALU.add),
                 reads=zk + [("xr", s2)], writes=[("z", s2)])
            for dh in range(2):
                P.op("dve", lambda e, s2=s2, dh=dh: e.bn_stats(out=st[:, dh, :], in_=zz[s2][:, dh * 512:(dh + 1) * 512]),
                     reads=[("z", s2)], writes=[("st", dh)])
            P.op("dve", lambda e: e.bn_aggr(out=mv[:, 0:2], in_=st[:].rearrange("p a b -> p (a b)")),
                 reads=[("st", 0), ("st", 1)], writes=["mv"])
            P.op("dve", lambda e: e.tensor_scalar(out=mv[:, 2:3], in0=mv[:, 1:2], scalar1=1e-5, scalar2=None, op0=ALU.add),
                 reads=["mv"], writes=["mv2"])
            P.op("act", lambda e: e.activation(out=mv[:, 2:3], in_=mv[:, 2:3], func=AF.Ln), reads=["mv2"], writes=["mv2"])
            P.op("act", lambda e: e.activation(out=mv[:, 3:4], in_=mv[:, 2:3], func=AF.Exp, scale=-0.5), reads=["mv2"], writes=["mv3"])
            P.op("dve", lambda e, s2=s2: e.tensor_scalar(out=zz[s2][:], in0=zz[s2][:], scalar1=mv[:, 0:1], scalar2=mv[:, 3:4],
                                                        op0=ALU.subtract, op1=ALU.mult),
                 reads=[("z", s2), "mv", "mv3"], writes=[("z", s2)])
            P.op("pool", lambda e, s2=s2: e.tensor_tensor(out=zz[s2][:], in0=zz[s2][:], in1=lg[:], op=ALU.mult),
                 reads=[("z", s2), "lg"], writes=[("z", s2)])
            P.op("pool", lambda e, s2=s2: e.tensor_tensor(out=zz[s2][:], in0=zz[s2][:], in1=lb[:], op=ALU.add),
                 reads=[("z", s2), "lb"], writes=[("z", s2)])
            P.op("sp", lambda e, s2=s2, r0=r0: e.dma_start(out=x_dst[r0:r0 + 128, :], in_=zz[s2][:]),
                 reads=[("z", s2)], writes=["xdst"], dma_key=tag + "zo%d" % s2)
        P.barrier()
    return [tag + "zo0", tag + "zo1"]


def moe_phase(nc, P, BK, cfg, tag, l, mod, idn, ones_f, msel_in, router_w, wg, wu, wd, x_src, h2T_d, moe_acc,
              affshare, affgath):
    NT = cfg.nt
    NTL = NT // 128
    bk = lambda i: ("bank", i)
    n_p, n_s = 2 * cfg.seq_p, 2 * cfg.seq_s
    cap_p, cap_s = 2 * n_p // 16, 2 * n_s // 16
    LP2 = 2 * cfg.lp
    with ExitStack() as ph:
        gates = ph.enter_context(nc.sbuf_tensor(tag + "gates", [128, NTL, 16], F32))
        ph2 = ExitStack()
        psb = lambda n, s, dt: ph2.enter_context(nc.sbuf_tensor(tag + n, s, dt))
        scsh = psb("scsh", [128, 4, 2, KC], F32)
        for b in range(4):
            P.op("sp", lambda e, b=b: e.dma_start(out=scsh[:, b, 1, :], in_=mod[l, b, 3 * D:4 * D].rearrange("(k p) -> p k", p=128),
                                                  allow_slow_non_contiguous=True), writes=[("scl", b, 1)], dma_key=tag + "c0")
            P.op("sp", lambda e, b=b: e.dma_start(out=scsh[:, b, 0, :], in_=mod[l, b, 4 * D:5 * D].rearrange("(k p) -> p k", p=128),
                                                  allow_slow_non_contiguous=True), writes=[("scl", b, 0)], dma_key=tag + "c0")
        P.op("dve", lambda e: e.tensor_scalar(out=scsh[:, :, 0, :], in0=scsh[:, :, 0, :], scalar1=1.0, scalar2=None, op0=ALU.add),
             reads=[("scl", b, jj) for b in range(4) for jj in range(2)], writes=["scsh"])
        rw = psb("rw", [128, KC, 16], F32)
        P.op("sp", lambda e: e.dma_start(out=rw[:], in_=router_w[l].rearrange("(k p) n -> p k n", p=128)), writes=["rw"], dma_key=tag + "c1")
        xin = [psb("xin%d" % i, [128, D], F32) for i in range(2)]
        hf = [psb("hf%d" % i, [128, KC, 128], F32) for i in range(2)]
        hb = [psb("hb%d" % i, [128, KC, 128], BF16) for i in range(2)]
        aff_all = psb("aff_all", [128, NTL, 16], F32)
        affT = psb("affT", [16, NT], F32)
        sm = psb("sm", [128, 4], F32)
        ex = psb("ex", [128, 16], F32)
        for i in range(NTL):
            b = seg_of_tile(cfg, i)
            s2 = i % 2
            r0 = i * 128
            P.op("sp", lambda e, s2=s2, r0=r0: e.dma_start(out=xin[s2][:], in_=x_src[r0:r0 + 128, :]), writes=[("xin", s2)],
                 dma_key=tag + "xin%d" % s2)
            for half in range(2):
                pp = 2 * s2 + half
                for jj in range(4):
                    k = half * 4 + jj
                    P.op("pe", lambda e, pp=pp, s2=s2, k=k, jj=jj: e.transpose(
                        out=BK[pp][:, jj * 128:(jj + 1) * 128], in_=xin[s2][:, k * 128:(k + 1) * 128], identity=idn[:]),
                        reads=[("xin", s2), "idn"], writes=[bk(pp)])
                for jj in range(4):
                    k = half * 4 + jj
                    P.op("act", lambda e, pp=pp, s2=s2, k=k, jj=jj, b=b: e.activation(
                        out=hf[s2][:, k, :], in_=BK[pp][:, jj * 128:(jj + 1) * 128], func=AF.Identity,
                        scale=scsh[:, b, 0, k:k + 1], bias=scsh[:, b, 1, k:k + 1]),
                        reads=[bk(pp), "scsh"], writes=[("hf", s2, k)])
                    P.op("dve", lambda e, s2=s2, k=k: e.tensor_copy(out=hb[s2][:, k, :], in_=hf[s2][:, k, :]),
                         reads=[("hf", s2, k)], writes=[("hb", s2, k)])
            P.op("sp", lambda e, s2=s2, r0=r0: e.dma_start(out=h2T_d[:, :, r0:r0 + 128].rearrange("k p t -> p k t"), in_=hb[s2][:]),
                 reads=[("hb", s2, k) for k in range(KC)], writes=["h2T_d"], dma_key=tag + "hb%d" % s2)
            pl = 4 + s2
            for k in range(KC):
                P.op("pe", lambda e, pl=pl, s2=s2, k=k: e.matmul(BK[pl][:, 0:16], lhsT=hf[s2][:, k, :], rhs=rw[:, k, :],
                                                                start=(k == 0), stop=(k == KC - 1)),
                     reads=[("hf", s2, kk) for kk in range(KC)] + ["rw"], writes=[bk(pl)])
            P.op("dve", lambda e, pl=pl: e.tensor_reduce(out=sm[:, 0:1], in_=BK[pl][:, 0:16], op=ALU.max, axis=mybir.AxisListType.X),
                 reads=[bk(pl)], writes=["sm0"])
            P.op("dve", lambda e: e.tensor_scalar(out=sm[:, 1:2], in0=sm[:, 0:1], scalar1=-1.0, scalar2=None, op0=ALU.mult),
                 reads=["sm0"], writes=["sm1"])
            P.op("act", lambda e, pl=pl: e.activation(out=ex[:], in_=BK[pl][:, 0:16], func=AF.Exp, bias=sm[:, 1:2]),
                 reads=[bk(pl), "sm1"], writes=["ex"])
            P.op("dve", lambda e: e.tensor_reduce(out=sm[:, 2:3], in_=ex[:], op=ALU.add, axis=mybir.AxisListType.X),
                 reads=["ex"], writes=["sm2"])
            P.op("dve", lambda e: e.reciprocal(out=sm[:, 3:4], in_=sm[:, 2:3]), reads=["sm2"], writes=["sm3"])
            P.op("dve", lambda e, i=i: e.tensor_scalar(out=aff_all[:, i, :], in0=ex[:], scalar1=sm[:, 3:4], scalar2=None, op0=ALU.mult),
                 reads=["ex", "sm3"], writes=[("aff", i)])
            pt_ = 6 + s2
            P.op("pe", lambda e, pt_=pt_, i=i: e.transpose(out=BK[pt_][0:16, 0:128], in_=aff_all[:, i, :], identity=idn[:]),
                 reads=[("aff", i), "idn"], writes=[bk(pt_)])
            P.op("act", lambda e, pt_=pt_, r0=r0: e.copy(out=affT[:, r0:r0 + 128], in_=BK[pt_][0:16, 0:128]),
                 reads=[bk(pt_)], writes=[("affT", i)])
        P.op("sp", lambda e: e.dma_start(out=affshare[:], in_=affT[:]), reads=[("affT", i) for i in range(NTL)],
             writes=["affshare"], dma_key=tag + "as")
        P.op("pool", lambda e: e.collective_compute("AllGather", ALU.bypass, replica_groups=[list(range(NCORES))],
                                                    ins=[affshare[:]], outs=[affgath[:]]),
             reads=["affshare"], writes=["affgath"], dma_key=tag + "ag", inc=1)
        T = psb("T", [128, NT], F32)
        junk = psb("junk", [128, NT], BF16)
        msel = psb("msel", [128, 128], F32)
        P.op("sp", lambda e: e.dma_start(out=T[:], in_=affgath[:]), reads=["affgath"], writes=["T"], dma_key=tag + "T")
        P.op("sp", lambda e: e.dma_start(out=msel[:], in_=msel_in[:]), writes=["msel"], dma_key=tag + "ms")
        bs = psb("bs", [128, 16], F32)
        P.op("dve", lambda e: e.memset(bs[:, 0:2], 0.0), writes=["lo"])
        P.op("dve", lambda e: e.memset(bs[:, 2:4], 1.0), writes=["hi"])
        P.op("dve", lambda e: e.memset(bs[:, 12:13], float(cap_p)), writes=["capp"])
        P.op("dve", lambda e: e.memset(bs[:, 13:14], float(cap_s)), writes=["caps"])
        for it in range(36):
            P.op("dve", lambda e: e.tensor_tensor(out=bs[:, 4:6], in0=bs[:, 0:2], in1=bs[:, 2:4], op=ALU.add), reads=["lo", "hi"], writes=["mid"])
            P.op("dve", lambda e: e.tensor_scalar(out=bs[:, 4:6], in0=bs[:, 4:6], scalar1=0.5, scalar2=None, op0=ALU.mult), reads=["mid"], writes=["mid"])
            P.op("dve", lambda e: e.memset(bs[:, 6:8], 0.0), writes=["cnt"])
            P.op("dve", lambda e: e.tensor_scalar(out=junk[:, 0:LP2], in0=T[:, 0:LP2], scalar1=bs[:, 4:5], scalar2=0.0,
                                                  op0=ALU.is_ge, op1=ALU.add, accum_out=bs[:, 6:7]),
                 reads=["T", "mid", "cnt"], writes=["cnt0", "junk"])
            P.op("dve", lambda e: e.tensor_scalar(out=junk[:, LP2:NT], in0=T[:, LP2:NT], scalar1=bs[:, 5:6], scalar2=0.0,
                                                  op0=ALU.is_ge, op1=ALU.add, accum_out=bs[:, 7:8]),
                 reads=["T", "mid", "cnt"], writes=["cnt1", "junk"])
            P.op("pe", lambda e: e.matmul(BK[0][:, 0:2], lhsT=msel[:, :], rhs=bs[:, 6:8], start=True, stop=True),
                 reads=["msel", "cnt0", "cnt1"], writes=[bk(0)])
            P.op("dve", lambda e: e.tensor_tensor(out=bs[:, 8:10], in0=BK[0][:, 0:2], in1=bs[:, 12:14], op=ALU.is_ge),
                 reads=[bk(0), "capp", "caps"], writes=["ge"])
            P.op("dve", lambda e: e.tensor_tensor(out=bs[:, 10:12], in0=bs[:, 8:10], in1=bs[:, 4:6], op=ALU.mult), reads=["ge", "mid"], writes=["t"])
            P.op("dve", lambda e: e.tensor_tensor(out=bs[:, 0:2], in0=bs[:, 0:2], in1=bs[:, 10:12], op=ALU.max), reads=["lo", "t"], writes=["lo"])
            P.op("dve", lambda e: e.scalar_tensor_tensor(out=bs[:, 10:12], in0=bs[:, 8:10], scalar=4.0, in1=bs[:, 4:6],
                                                         op0=ALU.mult, op1=ALU.add), reads=["ge", "mid", "lo"], writes=["t"])
            P.op("dve", lambda e: e.tensor_tensor(out=bs[:, 2:4], in0=bs[:, 2:4], in1=bs[:, 10:12], op=ALU.min), reads=["hi", "t"], writes=["hi"])
        dg = psb("dg", [16, 32], F32)
        thr_row = psb("thr_row", [128, 32], F32)
        for g in range(2):
            P.op("dve", lambda e, g=g: e.tensor_scalar(out=dg[:, g * 16:(g + 1) * 16], in0=idn[0:16, 0:16], scalar1=bs[0:16, g:g + 1],
                                                       scalar2=None, op0=ALU.mult), reads=["lo", "idn"], writes=[("dg", g)])
        P.op("pe", lambda e: e.matmul(BK[1][:, 0:32], lhsT=ones_f[0:16, :], rhs=dg[:, :], start=True, stop=True),
             reads=[("dg", 0), ("dg", 1), "ones_f"], writes=[bk(1)])
        P.op("dve", lambda e: e.tensor_copy(out=thr_row[:], in_=BK[1][:, 0:32]), reads=[bk(1)], writes=["thr_row"])
        for i in range(NTL):
            g = 0 if i * 128 < LP2 else 1
            P.op("dve", lambda e, i=i, g=g: e.tensor_tensor(out=gates[:, i, :], in0=aff_all[:, i, :], in1=thr_row[:, g * 16:(g + 1) * 16],
                                                           op=ALU.is_ge), reads=[("aff", i), "thr_row"], writes=[("gt", i)])
            P.op("dve", lambda e, i=i: e.tensor_tensor(out=gates[:, i, :], in0=gates[:, i, :], in1=aff_all[:, i, :], op=ALU.mult),
                 reads=[("gt", i), ("aff", i)], writes=[("gt", i)])
        P.barrier()
        ph2.close()
        psb = lambda n, s, dt: ph.enter_context(nc.sbuf_tensor(tag + n, s, dt))
        wgs2 = [psb("wgs%d" % i, [128, KC, FH], BF16) for i in range(2)]
        wus2 = [psb("wus%d" % i, [128, KC, FH], BF16) for i in range(2)]
        wds2 = [psb("wds%d" % i, [128, NFC, D], BF16) for i in range(2)]
        XT = [psb("XT%d" % i, [128, KC, 512], BF16) for i in range(2)]
        GT = psb("GT", [128, NFC, 512], BF16)
        sa = [psb("sa%d" % i, [128, 512], F32) for i in range(2)]
        acc = [psb("acc%d" % i, [128, D], F32) for i in range(2)]
        nblk = 0
        nacc = 0
        blocks = [(t0, min(512, NT - t0)) for t0 in range(0, NT, 512)]
        for e_ in range(16):
            for fh in range(2):
                f0 = fh * FH
                first = (e_ == 0 and fh == 0)
                wp = (e_ * 2 + fh) % 2
                wgs, wus, wds = wgs2[wp], wus2[wp], wds2[wp]
                for k in range(KC):
                    P.op("pool", lambda e, k=k, e_=e_, f0=f0, wgs=wgs: e.dma_start(out=wgs[:, k, :], in_=wg[l][e_ * D + k * 128:e_ * D + (k + 1) * 128, f0:f0 + FH]),
                         writes=[("wgs", wp, k)], dma_key=tag + "wg%d" % wp)
                    P.op("pool", lambda e, k=k, e_=e_, f0=f0, wus=wus: e.dma_start(out=wus[:, k, :], in_=wu[l][e_ * D + k * 128:e_ * D + (k + 1) * 128, f0:f0 + FH]),
                         writes=[("wus", wp, k)], dma_key=tag + "wu%d" % wp)
                for fc in range(NFC):
                    P.op("pool", lambda e, fc=fc, e_=e_, f0=f0, wds=wds: e.dma_start(out=wds[:, fc, :], in_=wd[l][e_ * 2816 + f0 + fc * 128:e_ * 2816 + f0 + (fc + 1) * 128, :]),
                         writes=[("wds", wp, fc)], dma_key=tag + "wd%d" % wp)
                wgk = [("wgs", wp, k) for k in range(KC)]
                wuk = [("wus", wp, k) for k in range(KC)]
                wdk = [("wds", wp, fc) for fc in range(NFC)]
                for (t0, tb) in blocks:
                    xs = nblk % 2
                    nblk += 1
                    P.op("sp", lambda e, xs=xs, t0=t0, tb=tb: e.dma_start(out=XT[xs][:, :, :tb], in_=h2T_d[:, :, t0:t0 + tb].rearrange("k p t -> p k t")),
                         reads=["h2T_d"], writes=[("XT", xs)], dma_key=tag + "XT%d" % xs)
                    for fc in range(NFC):
                        pa, pu = fc % 2, 2 + fc % 2
                        for k in range(KC):
                            P.op("pe", lambda e, pa=pa, xs=xs, k=k, fc=fc, tb=tb, wgs=wgs: e.matmul(
                                BK[pa][:, :tb], lhsT=wgs[:, k, fc * 128:(fc + 1) * 128], rhs=XT[xs][:, k, :tb],
                                start=(k == 0), stop=(k == KC - 1)), reads=wgk + [("XT", xs)], writes=[bk(pa)])
                        for k in range(KC):
                            P.op("pe", lambda e, pu=pu, xs=xs, k=k, fc=fc, tb=tb, wus=wus: e.matmul(
                                BK[pu][:, :tb], lhsT=wus[:, k, fc * 128:(fc + 1) * 128], rhs=XT[xs][:, k, :tb],
                                start=(k == 0), stop=(k == KC - 1)), reads=wuk + [("XT", xs)], writes=[bk(pu)])
                        P.op("act", lambda e, pa=pa, fc=fc, tb=tb: e.activation(out=sa[fc % 2][:, :tb], in_=BK[pa][:, :tb], func=AF.Silu),
                             reads=[bk(pa)], writes=[("sa", fc % 2)])
                        P.op("dve", lambda e, pu=pu, fc=fc, tb=tb: e.tensor_tensor(out=GT[:, fc, :tb], in0=BK[pu][:, :tb], in1=sa[fc % 2][:, :tb], op=ALU.mult),
                             reads=[bk(pu), ("sa", fc % 2)], writes=[("GT", fc)])
                    gtk = [("GT", fc) for fc in range(NFC)]
                    for tt in range(tb // 128):
                        ti = (t0 // 128) + tt
                        a2 = nacc % 2
                        nacc += 1
                        if not first:
                            P.op("sp", lambda e, a2=a2, ti=ti: e.dma_start(out=acc[a2][:], in_=moe_acc[ti * 128:(ti + 1) * 128, :]),
                                 reads=["moe_acc%d" % ti], writes=[("acc", a2)], dma_key=tag + "al%d" % a2)
                        for dh in range(2):
                            py = 4 + 2 * a2 + dh
                            for fc in range(NFC):
                                P.op("pe", lambda e, py=py, fc=fc, tt=tt, dh=dh, wds=wds: e.matmul(
                                    BK[py][:, :], lhsT=GT[:, fc, tt * 128:(tt + 1) * 128], rhs=wds[:, fc, dh * 512:(dh + 1) * 512],
                                    start=(fc == 0), stop=(fc == NFC - 1)), reads=gtk + wdk, writes=[bk(py)])
                            if first:
                                P.op("dve", lambda e, py=py, a2=a2, dh=dh, ti=ti, e_=e_: e.tensor_scalar(
                                    out=acc[a2][:, dh * 512:(dh + 1) * 512], in0=BK[py][:, :], scalar1=gates[:, ti, e_:e_ + 1], scalar2=None,
                                    op0=ALU.mult), reads=[bk(py), ("gt", ti)], writes=[("acc", a2, dh)])
                            else:
                                P.op("dve", lambda e, py=py, a2=a2, dh=dh, ti=ti, e_=e_: e.scalar_tensor_tensor(
                                    out=acc[a2][:, dh * 512:(dh + 1) * 512], in0=BK[py][:, :], scalar=gates[:, ti, e_:e_ + 1],
                                    in1=acc[a2][:, dh * 512:(dh + 1) * 512], op0=ALU.mult, op1=ALU.add),
                                    reads=[bk(py), ("gt", ti), ("acc", a2)], writes=[("acc", a2, dh)])
                        P.op("sp", lambda e, a2=a2, ti=ti: e.dma_start(out=moe_acc[ti * 128:(ti + 1) * 128, :], in_=acc[a2][:]),
                             reads=[("acc", a2, 0), ("acc", a2, 1)], writes=["moe_acc%d" % ti, ("acc", a2)], dma_key=tag + "as%d" % a2)
        P.barrier()


def l1_proj_phase(nc, P, BK, cfg, mod, idn, ones_f, x_src, w_in_odd, w_kr_sw, mla_q_norm, mla_kv_norm, mla_w_uq, mla_w_uq_sw,
                  mla_w_ukv, ropeC96, ropeS96, mlaQ, mlaKsh, mlaVsh, naQ, naKsh, naVsh):
    NT = cfg.nt
    bk = lambda i: ("bank", i)
    with ExitStack() as ph:
        psb = lambda n, s, dt: ph.enter_context(nc.sbuf_tensor("a1_" + n, s, dt))
        scsh = psb("scsh", [128, 4, 2, KC], F32)
        for b in range(4):
            P.op("sp", lambda e, b=b: e.dma_start(out=scsh[:, b, 1, :], in_=mod[1, b, 0:D].rearrange("(k p) -> p k", p=128),
                                                  allow_slow_non_contiguous=True), writes=[("scl", b, 1)], dma_key="a1c0")
            P.op("sp", lambda e, b=b: e.dma_start(out=scsh[:, b, 0, :], in_=mod[1, b, D:2 * D].rearrange("(k p) -> p k", p=128),
                                                  allow_slow_non_contiguous=True), writes=[("scl", b, 0)], dma_key="a1c0")
        P.op("dve", lambda e: e.tensor_scalar(out=scsh[:, :, 0, :], in0=scsh[:, :, 0, :], scalar1=1.0, scalar2=None, op0=ALU.add),
             reads=[("scl", b, jj) for b in range(4) for jj in range(2)], writes=["scsh"])
        wob = psb("wob", [128, KC, 1952], BF16)
        wks = psb("wks", [128, KC, 32], BF16)
        for k in range(KC):
            P.op("pool", lambda e, k=k: e.dma_start(out=wob[:, k, :], in_=w_in_odd[k * 128:(k + 1) * 128, :]), writes=[("wob", k)], dma_key="a1w")
            P.op("pool", lambda e, k=k: e.dma_start(out=wks[:, k, :], in_=w_kr_sw[k * 128:(k + 1) * 128, :]), writes=[("wks", k)], dma_key="a1w")
        wkeys = [("wob", k) for k in range(KC)] + [("wks", k) for k in range(KC)]
        wuq = psb("wuq", [128, 2, 768], BF16)
        wuqs = psb("wuqs", [128, 2, 768], BF16)
        wukv = psb("wukv", [128, 1024], BF16)
        for kc in range(2):
            P.op("pool", lambda e, kc=kc: e.dma_start(out=wuq[:, kc, :], in_=mla_w_uq[kc * 128:(kc + 1) * 128, :]), writes=[("wuq", kc)], dma_key="a1w")
            P.op("pool", lambda e, kc=kc: e.dma_start(out=wuqs[:, kc, :], in_=mla_w_uq_sw[kc * 128:(kc + 1) * 128, :]), writes=[("wuqs", kc)], dma_key="a1w")
        P.op("pool", lambda e: e.dma_start(out=wukv[:], in_=mla_w_ukv[:, :]), writes=["wukv"], dma_key="a1w")
        ukeys = [("wuq", 0), ("wuq", 1), ("wuqs", 0), ("wuqs", 1), "wukv"]
        qnm = psb("qnm", [128, 2], F32)
        kvnm = psb("kvnm", [128, 1], F32)
        P.op("sp", lambda e: e.dma_start(out=qnm[:], in_=mla_q_norm[:]), writes=["qnm"], dma_key="a1c1")
        P.op("sp", lambda e: e.dma_start(out=kvnm[:], in_=mla_kv_norm[:]), writes=["kvnm"], dma_key="a1c1")
        TB = 512
        xin = [psb("xin%d" % i, [128, D], F32) for i in range(2)]
        hT = [psb("hT%d" % i, [128, KC, TB], BF16) for i in range(2)]
        rc = psb("rc", [96, TB], F32)
        rs = psb("rs", [96, TB], F32)
        rck = psb("rck", [32, TB], F32)
        rsk = psb("rsk", [32, TB], F32)
        sq = psb("sq", [128, 3, TB], F32)
        rstd = psb("rstd", [128, 2, TB], F32)
        tmpf = psb("tmpf", [128, TB], F32)
        cqn = psb("cqn", [128, 2, TB], BF16)
        ckvn = psb("ckvn", [128, TB], BF16)
        t1 = [psb("t1%d" % i, [96, TB], F32) for i in range(2)]
        t2 = [psb("t2%d" % i, [96, TB], F32) for i in range(2)]
        qo = [psb("qo%d" % i, [128, TB], BF16) for i in range(2)]
        vaug = [psb("vaug%d" % i, [128, 8, 65], BF16) for i in range(2)]
        for i in range(2):
            P.op("dve", lambda e, i=i: e.memset(vaug[i][:], 1.0), writes=[("vaug", i)])
        nblk = nx = npt = nq = nv = 0
        for b, (off, ln, _S) in enumerate(cfg.segs):
            for t0 in range(0, ln, TB):
                tb = min(TB, ln - t0)
                hs = nblk % 2
                nblk += 1
                g0 = off + t0
                for tt in range(tb // 128):
                    xs = nx % 2
                    nx += 1
                    P.op("sp", lambda e, xs=xs, r=g0 + tt * 128: e.dma_start(out=xin[xs][:], in_=x_src[r:r + 128, :]),
                         writes=[("xin", xs)], dma_key="a1xin%d" % xs)
                    for half in range(2):
                        pp = npt % 2
                        npt += 1
                        for jj in range(4):
                            k = half * 4 + jj
                            P.op("pe", lambda e, pp=pp, xs=xs, k=k, jj=jj: e.transpose(
                                out=BK[pp][:, jj * 128:(jj + 1) * 128], in_=xin[xs][:, k * 128:(k + 1) * 128], identity=idn[:]),
                                reads=[("xin", xs), "idn"], writes=[bk(pp)])
                        for jj in range(4):
                            k = half * 4 + jj
                            P.op("act", lambda e, pp=pp, hs=hs, k=k, jj=jj, tt=tt, b=b: e.activation(
                                out=hT[hs][:, k, tt * 128:(tt + 1) * 128], in_=BK[pp][:, jj * 128:(jj + 1) * 128], func=AF.Identity,
                                scale=scsh[:, b, 0, k:k + 1], bias=scsh[:, b, 1, k:k + 1]),
                                reads=[bk(pp), "scsh"], writes=[("hT", hs, k, tt)])
                hk = [("hT", hs, k, tt) for k in range(KC) for tt in range(tb // 128)]
                P.op("sp", lambda e, g0=g0, tb=tb: e.dma_start(out=rc[:, :tb], in_=ropeC96[:, g0:g0 + tb]), writes=["rc"], dma_key="a1rc")
                P.op("sp", lambda e, g0=g0, tb=tb: e.dma_start(out=rs[:, :tb], in_=ropeS96[:, g0:g0 + tb]), writes=["rs"], dma_key="a1rs")

                def proj(bank, c0, ncol, wt=wob, tb=tb, hs=hs, hk=hk):
                    for k in range(KC):
                        P.op("pe", lambda e, bank=bank, k=k, c0=c0, ncol=ncol, wt=wt: e.matmul(
                            BK[bank][0:ncol, :tb], lhsT=wt[:, k, c0:c0 + ncol], rhs=hT[hs][:, k, :tb],
                            start=(k == 0), stop=(k == KC - 1)), reads=hk + wkeys, writes=[bk(bank)])

                proj(2, 0, 128)
                proj(3, 128, 128)
                proj(4, 256, 128)
                for j, bank in enumerate((2, 3, 4)):
                    P.op("act", lambda e, j=j, bank=bank, tb=tb: e.activation(out=sq[:, j, :tb], in_=BK[bank][:, :tb], func=AF.Square),
                         reads=[bk(bank)], writes=[("sq", j)])
                for j in range(2):
                    P.op("pe", lambda e, j=j, tb=tb: e.matmul(BK[5][:, :tb], lhsT=ones_f[:, :], rhs=sq[:, j, :tb], start=(j == 0), stop=(j == 1)),
                         reads=[("sq", j), "ones_f"], writes=[bk(5)])
                P.op("pe", lambda e, tb=tb: e.matmul(BK[6][:, :tb], lhsT=ones_f[:, :], rhs=sq[:, 2, :tb], start=True, stop=True),
                     reads=[("sq", 2), "ones_f"], writes=[bk(6)])
                for j, (bank, nf) in enumerate(((5, 256.0), (6, 128.0))):
                    P.op("dve", lambda e, j=j, bank=bank, nf=nf, tb=tb: e.tensor_scalar(out=rstd[:, j, :tb], in0=BK[bank][:, :tb], scalar1=1.0 / nf,
                                                                                      scalar2=1e-6, op0=ALU.mult, op1=ALU.add),
                         reads=[bk(bank)], writes=[("rstd", j)])
                    P.op("act", lambda e, j=j, tb=tb: e.activation(out=rstd[:, j, :tb], in_=rstd[:, j, :tb], func=AF.Ln), reads=[("rstd", j)], writes=[("rstd", j)])
                    P.op("act", lambda e, j=j, tb=tb: e.activation(out=rstd[:, j, :tb], in_=rstd[:, j, :tb], func=AF.Exp, scale=-0.5),
                         reads=[("rstd", j)], writes=[("rstd", j)])
                for j, bank in enumerate((2, 3)):
                    P.op("dve", lambda e, bank=bank, tb=tb: e.tensor_tensor(out=tmpf[:, :tb], in0=BK[bank][:, :tb], in1=rstd[:, 0, :tb], op=ALU.mult),
                         reads=[bk(bank), ("rstd", 0)], writes=["tmpf"])
                    P.op("act", lambda e, j=j, tb=tb: e.activation(out=cqn[:, j, :tb], in_=tmpf[:, :tb], func=AF.Identity, scale=qnm[:, j:j + 1]),
                         reads=["tmpf", "qnm"], writes=[("cqn", j)])
                P.op("dve", lambda e, tb=tb: e.tensor_tensor(out=tmpf[:, :tb], in0=BK[4][:, :tb], in1=rstd[:, 1, :tb], op=ALU.mult),
                     reads=[bk(4), ("rstd", 1)], writes=["tmpf"])
                P.op("act", lambda e, tb=tb: e.activation(out=ckvn[:, :tb], in_=tmpf[:, :tb], func=AF.Identity, scale=kvnm[:, 0:1]),
                     reads=["tmpf", "kvnm"], writes=["ckvn"])
                for h in range(8):
                    qs = nq % 2
                    nq += 1
                    for (bank, wt_) in ((2 + qs, wuq), (4 + qs, wuqs)):
                        for kc in range(2):
                            P.op("pe", lambda e, bank=bank, wt_=wt_, kc=kc, h=h, tb=tb: e.matmul(
                                BK[bank][0:96, :tb], lhsT=wt_[:, kc, h * 96:(h + 1) * 96], rhs=cqn[:, kc, :tb], start=(kc == 0), stop=(kc == 1)),
                                reads=[("cqn", 0), ("cqn", 1)] + ukeys, writes=[bk(bank)])
                    P.op("dve", lambda e, qs=qs, tb=tb: e.tensor_tensor(out=t1[qs][:, :tb], in0=BK[2 + qs][0:96, :tb], in1=rc[:, :tb], op=ALU.mult),
                         reads=[bk(2 + qs), "rc"], writes=[("t1", qs)])
                    P.op("dve", lambda e, qs=qs, tb=tb: e.tensor_tensor(out=t2[qs][:, :tb], in0=BK[4 + qs][0:96, :tb], in1=rs[:, :tb], op=ALU.mult),
                         reads=[bk(4 + qs), "rs"], writes=[("t2", qs)])
                    P.op("pool", lambda e, qs=qs, tb=tb: e.tensor_tensor(out=qo[qs][0:96, :tb], in0=t1[qs][:, :tb], in1=t2[qs][:, :tb], op=ALU.add),
                         reads=[("t1", qs), ("t2", qs)], writes=[("qo", qs)])
                    P.op("sp", lambda e, qs=qs, h=h, g0=g0, tb=tb: e.dma_start(out=mlaQ[h, :, g0:g0 + tb], in_=qo[qs][0:96, :tb]),
                         reads=[("qo", qs)], writes=["mlaQ"], dma_key="a1qo%d" % qs)
                qs = nq % 2
                nq += 1
                proj(2 + qs, 384, 32)
                proj(4 + qs, 0, 32, wt=wks)
                P.op("sp", lambda e, g0=g0, tb=tb: e.dma_start(out=rck[:, :tb], in_=ropeC96[64:96, g0:g0 + tb]), writes=["rck"], dma_key="a1rck")
                P.op("sp", lambda e, g0=g0, tb=tb: e.dma_start(out=rsk[:, :tb], in_=ropeS96[64:96, g0:g0 + tb]), writes=["rsk"], dma_key="a1rsk")
                P.op("dve", lambda e, qs=qs, tb=tb: e.tensor_tensor(out=t1[qs][0:32, :tb], in0=BK[2 + qs][0:32, :tb], in1=rck[:, :tb], op=ALU.mult),
                     reads=[bk(2 + qs), "rck"], writes=[("t1", qs)])
                P.op("dve", lambda e, qs=qs, tb=tb: e.tensor_tensor(out=t2[qs][0:32, :tb], in0=BK[4 + qs][0:32, :tb], in1=rsk[:, :tb], op=ALU.mult),
                     reads=[bk(4 + qs), "rsk"], writes=[("t2", qs)])
                P.op("pool", lambda e, qs=qs, tb=tb: e.tensor_tensor(out=qo[qs][0:32, :tb], in0=t1[qs][0:32, :tb], in1=t2[qs][0:32, :tb], op=ALU.add),
                     reads=[("t1", qs), ("t2", qs)], writes=[("qo", qs)])
                for h in range(8):
                    P.op("sp", lambda e, qs=qs, h=h, g0=g0, tb=tb: e.dma_start(out=mlaKsh[h * 96 + 64:h * 96 + 96, g0:g0 + tb], in_=qo[qs][0:32, :tb]),
                         reads=[("qo", qs)], writes=["mlaKsh"], dma_key="a1qo%d" % qs)
                for kh in range(4):
                    qs = nq % 2
                    nq += 1
                    P.op("pe", lambda e, qs=qs, kh=kh, tb=tb: e.matmul(BK[2 + qs][:, :tb], lhsT=wukv[:, kh * 128:(kh + 1) * 128], rhs=ckvn[:, :tb],
                                                                  start=True, stop=True), reads=["ckvn"] + ukeys, writes=[bk(2 + qs)])
                    P.op("act", lambda e, qs=qs, tb=tb: e.copy(out=qo[qs][:, :tb], in_=BK[2 + qs][:, :tb]), reads=[bk(2 + qs)], writes=[("qo", qs)])
                    for hh in range(2):
                        h = 2 * kh + hh
                        P.op("sp", lambda e, qs=qs, h=h, hh=hh, g0=g0, tb=tb: e.dma_start(
                            out=mlaKsh[h * 96:h * 96 + 64, g0:g0 + tb], in_=qo[qs][hh * 64:(hh + 1) * 64, :tb]),
                            reads=[("qo", qs)], writes=["mlaKsh"], dma_key="a1qo%d" % qs)
                for tt in range(tb // 128):
                    vs = nv % 2
                    nv += 1
                    P.op("pe", lambda e, vs=vs, tt=tt: e.matmul(BK[6 + vs][:, :], lhsT=ckvn[:, tt * 128:(tt + 1) * 128], rhs=wukv[:, 512:1024],
                                                               start=True, stop=True), reads=["ckvn"] + ukeys, writes=[bk(6 + vs)])
                    P.op("act", lambda e, vs=vs: e.copy(out=vaug[vs][:, :, 0:64], in_=BK[6 + vs][:, :].rearrange("p (h d) -> p h d", d=64)),
                         reads=[bk(6 + vs)], writes=[("vaug", vs)])
                    P.op("sp", lambda e, vs=vs, r=g0 + tt * 128: e.dma_start(out=mlaVsh[r:r + 128, :], in_=vaug[vs][:].rearrange("p h d -> p (h d)")),
                         reads=[("vaug", vs)], writes=["mlaVsh"], dma_key="a1va%d" % vs)
                for c in range(4):
                    qs = nq % 2
                    nq += 1
                    proj(2 + qs, 416 + c * 128, 128)
                    P.op("act", lambda e, qs=qs, tb=tb: e.copy(out=qo[qs][:, :tb], in_=BK[2 + qs][:, :tb]), reads=[bk(2 + qs)], writes=[("qo", qs)])
                    P.op("sp", lambda e, qs=qs, c=c, g0=g0, tb=tb: e.dma_start(out=naQ[c, :, g0:g0 + tb], in_=qo[qs][:, :tb]),
                         reads=[("qo", qs)], writes=["naQ"], dma_key="a1qo%d" % qs)
                for c in range(4):
                    qs = nq % 2
                    nq += 1
                    proj(2 + qs, 928 + c * 128, 128)
                    P.op("act", lambda e, qs=qs, tb=tb: e.copy(out=qo[qs][:, :tb], in_=BK[2 + qs][:, :tb]), reads=[bk(2 + qs)], writes=[("qo", qs)])
                    P.op("sp", lambda e, qs=qs, c=c, g0=g0, tb=tb: e.dma_start(
                        out=naKsh[c][g0:g0 + tb, :].rearrange("(j p) x -> p j x", p=128),
                        in_=qo[qs][:, :tb].rearrange("p (j x) -> p j x", x=128)),
                        reads=[("qo", qs)], writes=["naKsh"], dma_key="a1qo%d" % qs)
                for tt in range(tb // 128):
                    vs = nv % 2
                    nv += 1
                    for k in range(KC):
                        P.op("pe", lambda e, vs=vs, k=k, tt=tt, hs=hs: e.matmul(BK[6 + vs][:, :], lhsT=hT[hs][:, k, tt * 128:(tt + 1) * 128],
                                                                             rhs=wob[:, k, 1440:1952], start=(k == 0), stop=(k == KC - 1)),
                             reads=hk + wkeys, writes=[bk(6 + vs)])
                    P.op("act", lambda e, vs=vs: e.copy(out=vaug[vs][:, :, 0:64], in_=BK[6 + vs][:, :].rearrange("p (h d) -> p h d", d=64)),
                         reads=[bk(6 + vs)], writes=[("vaug", vs)])
                    for cp in range(4):
                        P.op("sp", lambda e, vs=vs, cp=cp, r=g0 + tt * 128: e.dma_start(
                            out=naVsh[cp][r:r + 128, :], in_=vaug[vs][:, 2 * cp:2 * cp + 2, :].rearrange("p h d -> p (h d)")),
                            reads=[("vaug", vs)], writes=["naVsh"], dma_key="a1va%d" % vs)
        P.barrier()


def mla_attn_phase(nc, P, BK, cfg, ones_f, mlaQ, mlaKg, mlaVg, attn_dst):
    NT = cfg.nt
    bk = lambda i: ("bank", i)
    with ExitStack() as ph:
        psb = lambda n, s, dt: ph.enter_context(nc.sbuf_tensor("b1_" + n, s, dt))
        SMAX = max(sg[2] for sg in cfg.segs)
        LMAX = max(sg[1] for sg in cfg.segs)
        KT = psb("KT", [96, SMAX], BF16)
        VV = psb("VV", [128, SMAX // 128, 65], BF16)
        QT = psb("QT", [96, LMAX], BF16)
        QB = 512
        eb = [psb("eb%d" % i, [128, QB], BF16) for i in range(2)]
        rz = psb("rz", [128, QB], F32)
        ob = [psb("ob%d" % i, [64, QB], BF16) for i in range(2)]
        scale = 96 ** -0.5
        nst = nob = 0
        for b, (off, ln, S) in enumerate(cfg.segs):
            nchunk = S // 128
            for h in range(8):
                for r in range(NCORES):
                    P.op("sp", lambda e, r=r, h=h, off=off, ln=ln: e.dma_start(
                        out=KT[:, r * ln:(r + 1) * ln], in_=mlaKg[r * 768 + h * 96:r * 768 + (h + 1) * 96, off:off + ln]),
                        reads=["mlaKg"], writes=[("KT", r)], dma_key="b1KT")
                    P.op("sp", lambda e, r=r, h=h, off=off, ln=ln: e.dma_start(
                        out=VV[:, r * (ln // 128):(r + 1) * (ln // 128), :],
                        in_=mlaVg[r * NT + off:r * NT + off + ln, h * 65:(h + 1) * 65].rearrange("(c p) d -> p c d", p=128)),
                        reads=["mlaVg"], writes=[("VV", r)], dma_key="b1VV")
                P.op("sp", lambda e, h=h, off=off, ln=ln: e.dma_start(out=QT[:, :ln], in_=mlaQ[h, :, off:off + ln]), writes=["QT"], dma_key="b1QT")
                kv_keys = [("KT", r) for r in range(NCORES)] + [("VV", r) for r in range(NCORES)]
                for q0 in range(0, ln, QB):
                    qb = min(QB, ln - q0)
                    pO = 4 + (nob % 2)
                    for c in range(nchunk):
                        si = nst % 2
                        nst += 1
                        P.op("pe", lambda e, si=si, c=c, q0=q0, qb=qb: e.matmul(
                            BK[si][:, :qb], lhsT=KT[:, c * 128:(c + 1) * 128], rhs=QT[:, q0:q0 + qb], start=True, stop=True),
                            reads=kv_keys + ["QT"], writes=[bk(si)])
                        P.op("act", lambda e, si=si, qb=qb: e.activation(out=eb[si][:, :qb], in_=BK[si][:, :qb], func=AF.Exp, scale=scale),
                             reads=[bk(si)], writes=[("eb", si)])
                        P.op("pe", lambda e, si=si, c=c, qb=qb, nchunk=nchunk, pO=pO: e.matmul(
                            BK[pO][0:65, :qb], lhsT=VV[:, c, :], rhs=eb[si][:, :qb], start=(c == 0), stop=(c == nchunk - 1)),
                            reads=kv_keys + [("eb", si)], writes=[bk(pO)])
                    P.op("dve", lambda e, pO=pO, qb=qb: e.reciprocal(out=rz[64:65, :qb], in_=BK[pO][64:65, :qb]), reads=[bk(pO)], writes=["rz"])
                    P.op("pe", lambda e, qb=qb: e.matmul(BK[3][0:64, :qb], lhsT=ones_f[64:65, 0:64], rhs=rz[64:65, :qb], start=True, stop=True),
                         reads=["rz", "ones_f"], writes=[bk(3)])
                    P.op("dve", lambda e, qb=qb: e.tensor_copy(out=rz[0:64, :qb], in_=BK[3][0:64, :qb]), reads=[bk(3)], writes=["rzb"])
                    os_ = nob % 2
                    nob += 1
                    P.op("dve", lambda e, pO=pO, os_=os_, qb=qb: e.tensor_tensor(out=ob[os_][:, :qb], in0=BK[pO][0:64, :qb], in1=rz[0:64, :qb], op=ALU.mult),
                         reads=[bk(pO), "rzb"], writes=[("ob", os_)])
                    P.op("sp", lambda e, os_=os_, h=h, g=off + q0, qb=qb: e.dma_start(
                        out=attn_dst[h // 2, (h % 2) * 64:(h % 2) * 64 + 64, g:g + qb], in_=ob[os_][:, :qb]),
                        reads=[("ob", os_)], writes=["attn1"], dma_key="b1ob%d" % os_)
        P.barrier()
    return ["b1ob0", "b1ob1"]


def na_attn_phase(nc, P, BK, cfg, ones_f, naQ, naKsh, naVsh, naKg, naVg, na_bt, na_rmask, idx_n, attn_dst):
    NT = cfg.nt
    bk = lambda i: ("bank", i)
    with ExitStack() as ph:
        psb = lambda n, s, dt: ph.enter_context(nc.sbuf_tensor("n1_" + n, s, dt))
        LMAX = max(sg[1] for sg in cfg.segs)
        NBM = LMAX // 128
        bt = psb("bt", [128, 8, 9, 128], BF16)
        P.op("sp", lambda e: e.dma_start(out=bt[:].rearrange("p h c q -> p (h c q)"), in_=na_bt[:, :]), writes=["bt"], dma_key="n1c0")
        rm = psb("rm", [128, NT // 128, 9, 2], F32)
        P.op("sp", lambda e: e.dma_start(out=rm[:].rearrange("p m c q -> p (m c q)"), in_=na_rmask[:, :]), writes=["rm"], dma_key="n1c1")
        idt = psb("idt", [128, 32], I32)
        P.op("sp", lambda e: e.dma_start(out=idt[:], in_=idx_n[:, :]), writes=["idt"], dma_key="n1c2")
        KTn = [psb("KTn%d" % c, [128, (NBM + 8) * 128], BF16) for c in range(4)]
        Vn = psb("Vn", [128, NBM + 8, 520], BF16)
        QTn = psb("QTn", [128, 4, LMAX], BF16)
        tt_ = [psb("tt%d" % i, [128, 128], F32) for i in range(2)]
        et = [psb("et%d" % i, [128, 128], BF16) for i in range(2)]
        rz = psb("rz", [128, 128], F32)
        ob = [psb("ob%d" % i, [64, 128], BF16) for i in range(2)]
        scale = 64 ** -0.5
        nq = nt = nob = 0
        pair0 = 0
        for b, (off, ln, S) in enumerate(cfg.segs):
            nb = ln // 128
            for c in range(4):
                P.op("sp", lambda e, c=c, off=off, ln=ln: e.dma_start(
                    out=KTn[c][:, 512:512 + ln].rearrange("p (j x) -> p j x", x=128),
                    in_=naKsh[c][off:off + ln, :].rearrange("(j p) x -> p j x", p=128)),
                    writes=[("KTl", c)], dma_key="n1KT")
                P.op("sp", lambda e, c=c, off=off, ln=ln, nb=nb: e.dma_start(
                    out=Vn[:, 4:4 + nb, c * 130:(c + 1) * 130], in_=naVsh[c][off:off + ln, :].rearrange("(j p) x -> p j x", p=128)),
                    writes=[("Vl", c)], dma_key="n1V")
                P.op("sp", lambda e, c=c, off=off, ln=ln: e.dma_start(out=QTn[:, c, :ln], in_=naQ[c, :, off:off + ln]),
                     writes=[("QTn", c)], dma_key="n1QT")
            for hb in range(8):
                eb_ = hb if hb < 4 else nb + hb
                col = b * 8 + hb
                for c in range(4):
                    P.op("pool", lambda e, c=c, eb_=eb_, col=col: e.indirect_dma_start(
                        out=KTn[c][:, eb_ * 128:(eb_ + 1) * 128], out_offset=None, in_=naKg[c][:, :],
                        in_offset=bass.IndirectOffsetOnAxis(ap=idt[:, col:col + 1], axis=0)),
                        reads=["idt"], writes=[("KTg", hb, c)], dma_key="n1g%d" % (hb % 2))
                    P.op("pool", lambda e, c=c, eb_=eb_, col=col: e.indirect_dma_start(
                        out=Vn[:, eb_, c * 130:(c + 1) * 130], out_offset=None, in_=naVg[c][:, :],
                        in_offset=bass.IndirectOffsetOnAxis(ap=idt[:, col:col + 1], axis=0)),
                        reads=["idt"], writes=[("Vg", hb, c)], dma_key="n1g%d" % (hb % 2))
            kvk = [("KTl", c) for c in range(4)] + [("QTn", c) for c in range(4)] + [("Vl", c) for c in range(4)] + \
                  [("KTg", hb, c) for hb in range(8) for c in range(4)] + [("Vg", hb, c) for hb in range(8) for c in range(4)]
            for m in range(nb):
                pm = pair0 + m
                for h in range(8):
                    c, hh = h // 2, h % 2
                    pO = 4 + (nob % 2)
                    for kc in range(9):
                        qs = nq % 4
                        nq += 1
                        P.op("pe", lambda e, qs=qs, c=c, hh=hh, m=m, kc=kc: e.matmul(
                            BK[qs][:, 0:128], lhsT=KTn[c][hh * 64:(hh + 1) * 64, (m + kc) * 128:(m + kc + 1) * 128],
                            rhs=QTn[hh * 64:(hh + 1) * 64, c, m * 128:(m + 1) * 128], start=True, stop=True),
                            reads=kvk, writes=[bk(qs)])
                        ts_ = nt % 2
                        nt += 1
                        P.op("dve", lambda e, qs=qs, ts_=ts_, h=h, kc=kc: e.scalar_tensor_tensor(
                            out=tt_[ts_][:], in0=BK[qs][:, 0:128], scalar=scale, in1=bt[:, h, kc, :], op0=ALU.mult, op1=ALU.add),
                            reads=[bk(qs), "bt"], writes=[("tt", ts_)])
                        for qq in range(2):
                            P.op("act", lambda e, ts_=ts_, qq=qq, pm=pm, kc=kc: e.activation(
                                out=et[ts_][:, qq * 64:(qq + 1) * 64], in_=tt_[ts_][:, qq * 64:(qq + 1) * 64], func=AF.Exp,
                                bias=rm[:, pm, kc, qq:qq + 1]),
                                reads=[("tt", ts_), "rm"], writes=[("et", ts_, qq)])
                        P.op("pe", lambda e, ts_=ts_, m=m, kc=kc, h=h, pO=pO: e.matmul(
                            BK[pO][0:65, 0:128], lhsT=Vn[:, m + kc, h * 65:(h + 1) * 65], rhs=et[ts_][:], start=(kc == 0), stop=(kc == 8)),
                            reads=kvk + [("et", ts_, 0), ("et", ts_, 1)], writes=[bk(pO)])
                    P.op("dve", lambda e, pO=pO: e.reciprocal(out=rz[64:65, :], in_=BK[pO][64:65, 0:128]), reads=[bk(pO)], writes=["rz"])
                    P.op("pe", lambda e: e.matmul(BK[6][0:64, 0:128], lhsT=ones_f[64:65, 0:64], rhs=rz[64:65, :], start=True, stop=True),
                         reads=["rz", "ones_f"], writes=[bk(6)])
                    P.op("dve", lambda e: e.tensor_copy(out=rz[0:64, :], in_=BK[6][0:64, 0:128]), reads=[bk(6)], writes=["rzb"])
                    os_ = nob % 2
                    nob += 1
                    P.op("dve", lambda e, pO=pO, os_=os_: e.tensor_tensor(out=ob[os_][:, :], in0=BK[pO][0:64, 0:128], in1=rz[0:64, :], op=ALU.mult),
                         reads=[bk(pO), "rzb"], writes=[("ob", os_)])
                    P.op("sp", lambda e, os_=os_, h=h, g=off + m * 128: e.dma_start(
                        out=attn_dst[4 + h // 2, (h % 2) * 64:(h % 2) * 64 + 64, g:g + 128], in_=ob[os_][:, :]),
                        reads=[("ob", os_)], writes=["attn1n"], dma_key="n1ob%d" % os_)
            pair0 += nb
        P.barrier()
    return ["n1ob0", "n1ob1"]


def build(cfg, debug_outs=False, stop_after=None):
    nc = bass.Bass("TRN2", target_bir_lowering=False)
    NT = cfg.nt
    din = lambda n, s, dt=F32: nc.dram_tensor(n, s, dt, kind="ExternalInput").ap()
    x_loc = din("x_loc", [NT, D])
    c_all = din("c_all", [4, D])
    ident = din("ident", [128, 128])
    ropeC = din("ropeC", [128, NT])
    ropeS = din("ropeS", [128, NT])
    w_in_even = din("w_in_even", [D, EVEN_IN])
    w_in_even_sw = din("w_in_even_sw", [D, EVEN_IN])
    ada_w = din("ada_w", [2, D, 6 * D])
    ada_b = din("ada_b", [2, 6 * D])
    diff_lambda = din("diff_lambda", [1, 256])
    diff_subln = din("diff_subln", [128, 1])
    swa_sink = din("swa_sink", [1, 8])
    w_out_even = din("w_out_even", [D, D])
    ln_g = din("ln_g", [2, 2, D])
    ln_b = din("ln_b", [2, 2, D])
    router_w = din("router_w", [2, D, 16])
    wg_sh = din("wg_sh", [2, 2 * D, 2816])
    wu_sh = din("wu_sh", [2, 2 * D, 2816])
    wd_sh = din("wd_sh", [2, 2 * 2816, D])
    wg_bn = [nc.dram_tensor("wg_bn%d" % l, [2 * D, 2816], F32).ap() for l in range(2)]
    wu_bn = [nc.dram_tensor("wu_bn%d" % l, [2 * D, 2816], F32).ap() for l in range(2)]
    wd_bn = [nc.dram_tensor("wd_bn%d" % l, [2 * 2816, D], F32).ap() for l in range(2)]
    wg_f = [nc.dram_tensor("wg_f%d" % l, [16 * D, 2816], F32).ap() for l in range(2)]
    wu_f = [nc.dram_tensor("wu_f%d" % l, [16 * D, 2816], F32).ap() for l in range(2)]
    wd_f = [nc.dram_tensor("wd_f%d" % l, [16 * 2816, D], F32).ap() for l in range(2)]
    msel_in = din("msel", [128, 128])
    w_in_odd = din("w_in_odd", [D, 1952])
    w_kr_sw = din("w_kr_sw", [D, 32])
    mla_q_norm = din("mla_q_norm", [128, 2])
    mla_kv_norm = din("mla_kv_norm", [128, 1])
    mla_w_uq = din("mla_w_uq", [256, 768])
    mla_w_uq_sw = din("mla_w_uq_sw", [256, 768])
    mla_w_ukv = din("mla_w_ukv", [128, 1024])
    ropeC96 = din("ropeC96", [96, NT])
    ropeS96 = din("ropeS96", [96, NT])
    w_out_odd = din("w_out_odd", [D, D])
    na_bt = din("na_bt", [128, 8 * 9 * 128], BF16)
    na_rmask = din("na_rmask", [128, (NT // 128) * 18])
    idx_n = nc.dram_tensor("idx_n", [128, 32], I32, kind="ExternalInput").ap()
    wmask = din("wmask", [2, 128, 512], BF16)
    idx_w = nc.dram_tensor("idx_w", [4, 2, 128, 1], I32, kind="ExternalInput").ap()
    y_loc = nc.dram_tensor("y_loc", [NT, D], F32, kind="ExternalOutput").ap()
    dbgk = "ExternalOutput" if debug_outs else "Internal"
    mod = nc.dram_tensor("mod", [2, 4, 6 * D], F32, kind=dbgk).ap()
    projT = nc.dram_tensor("projT", [18, 128, NT], BF16).ap()
    kshare = nc.dram_tensor("kshare", [4 * 128, NT], BF16).ap()
    vshare = nc.dram_tensor("vshare", [NT, 512], BF16).ap()
    kgath = nc.dram_tensor("kgath", [NCORES * 4 * 128, NT], BF16).ap()
    vgath = nc.dram_tensor("vgath", [NCORES * NT, 512], BF16).ap()
    kbshare = nc.dram_tensor("kbshare", [NT, 128], BF16).ap()
    vbshare = nc.dram_tensor("vbshare", [NT, 130], BF16).ap()
    kbgath = nc.dram_tensor("kbgath", [NCORES * NT, 128], BF16).ap()
    vbgath = nc.dram_tensor("vbgath", [NCORES * NT, 130], BF16).ap()
    xa = nc.dram_tensor("xa", [NT, D], F32, kind=dbgk).ap()
    xb = nc.dram_tensor("xb", [NT, D], F32, kind=dbgk).ap()
    h2T_d = nc.dram_tensor("h2T_d", [KC, 128, NT], BF16).ap()
    moe_acc = nc.dram_tensor("moe_acc", [NT, D], F32).ap()
    affshare = nc.dram_tensor("affshare", [16, NT], F32).ap()
    affgath = nc.dram_tensor("affgath", [NCORES * 16, NT], F32).ap()
    mlaQ = nc.dram_tensor("mlaQ", [8, 96, NT], BF16).ap()
    mlaKsh = nc.dram_tensor("mlaKsh", [8 * 96, NT], BF16).ap()
    mlaVsh = nc.dram_tensor("mlaVsh", [NT, 520], BF16).ap()
    mlaKg = nc.dram_tensor("mlaKg", [NCORES * 8 * 96, NT], BF16).ap()
    mlaVg = nc.dram_tensor("mlaVg", [NCORES * NT, 520], BF16).ap()
    naQ = nc.dram_tensor("naQ", [4, 128, NT], BF16).ap()
    naKsh = [nc.dram_tensor("naKsh%d" % c, [NT, 128], BF16).ap() for c in range(4)]
    naVsh = [nc.dram_tensor("naVsh%d" % c, [NT, 130], BF16).ap() for c in range(4)]
    naKg = [nc.dram_tensor("naKg%d" % c, [NCORES * NT, 128], BF16).ap() for c in range(4)]
    naVg = [nc.dram_tensor("naVg%d" % c, [NCORES * NT, 130], BF16).ap() for c in range(4)]
    attnT1 = nc.dram_tensor("attnT1", [8, 128, NT], BF16, kind=dbgk).ap()
    attnT = nc.dram_tensor("attnT", [8, 128, NT], BF16, kind=dbgk).ap()

    P = Prog(nc)
    with ExitStack() as es:
        sb = lambda n, s, dt: es.enter_context(nc.sbuf_tensor(n, s, dt))
        BK = [es.enter_context(nc.psum_tensor("bank%d" % i, [128, 512], F32)) for i in range(8)]
        bk = lambda i: ("bank", i)
        idn = sb("idn", [128, 128], F32)
        ones_bf = sb("ones_bf", [128, 128], BF16)
        ones_f = sb("ones_f", [128, 128], F32)
        P.op("sp", lambda e: e.dma_start(out=idn[:], in_=ident[:]), writes=["idn"], dma_key="c0")
        P.op("dve", lambda e: e.memset(ones_bf[:], 1.0), writes=["ones_bf"])
        P.op("dve", lambda e: e.memset(ones_f[:], 1.0), writes=["ones_f"])

        for l in range(2):
            for (sh, bn, ff, nm) in ((wg_sh, wg_bn, wg_f, "g"), (wu_sh, wu_bn, wu_f, "u"), (wd_sh, wd_bn, wd_f, "d")):
                nrows = sh.shape[1]
                for r0 in range(0, nrows, 512):
                    r1 = min(nrows, r0 + 512)
                    P.op("sp", lambda e, sh=sh, bn=bn, l=l, r0=r0, r1=r1: e.dma_start(out=bn[l][r0:r1, :], in_=sh[l, r0:r1, :]),
                         writes=[("bn", nm, l, r0)], dma_key="wbn%s%d" % (nm, l))
                P.op("pool", lambda e, bn=bn, ff=ff, l=l: e.collective_compute(
                    "AllGather", ALU.bypass, replica_groups=[list(range(NCORES))], ins=[bn[l][:]], outs=[ff[l][:]]),
                    reads=[("bn", nm, l, r0) for r0 in range(0, sh.shape[1], 512)], writes=[("wf", nm, l)], dma_key="agw", inc=1)

        with ExitStack() as ph:
            psb = lambda n, s, dt: ph.enter_context(nc.sbuf_tensor(n, s, dt))
            cT = psb("cT", [128, KC, 4], F32)
            for b in range(4):
                P.op("sp", lambda e, b=b: e.dma_start(out=cT[:, :, b], in_=c_all[b, :].rearrange("(k p) -> p k", p=128),
                                                      allow_slow_non_contiguous=True),
                     writes=[("cTl", b)], dma_key="c1")
            P.op("act", lambda e: e.activation(out=cT[:], in_=cT[:], func=AF.Silu),
                 reads=[("cTl", b) for b in range(4)], writes=["cT"])
            wa = [psb("wa%d" % i, [128, KC, 512], F32) for i in range(2)]
            ba = [psb("ba%d" % i, [4, 512], F32) for i in range(2)]
            mo = [psb("mo%d" % i, [4, 512], F32) for i in range(2)]
            it = 0
            for l in range(2):
                for cc in range(12):
                    s = it % 2
                    it += 1
                    P.op("sp", lambda e, s=s, l=l, cc=cc: e.dma_start(
                        out=wa[s][:], in_=ada_w[l, :, cc * 512:(cc + 1) * 512].rearrange("(k p) n -> p k n", p=128)),
                        writes=[("wa", s)], dma_key="wa%d" % s)
                    P.op("sp", lambda e, s=s, l=l, cc=cc: e.dma_start(
                        out=ba[s][:], in_=ada_b[l:l + 1, cc * 512:(cc + 1) * 512].broadcast_to([4, 512])),
                        writes=[("ba", s)], dma_key="ba%d" % s)
                    for k in range(KC):
                        P.op("pe", lambda e, s=s, k=k: e.matmul(BK[s][0:4, :], lhsT=cT[:, k, :], rhs=wa[s][:, k, :],
                                                                start=(k == 0), stop=(k == KC - 1)),
                             reads=[("wa", s), "cT"], writes=[bk(s)])
                    P.op("dve", lambda e, s=s: e.tensor_tensor(out=mo[s][:], in0=BK[s][0:4, :], in1=ba[s][:], op=ALU.add),
                         reads=[bk(s), ("ba", s)], writes=[("mo", s)])
                    P.op("sp", lambda e, s=s, l=l, cc=cc: e.dma_start(out=mod[l, :, cc * 512:(cc + 1) * 512], in_=mo[s][:]),
                         reads=[("mo", s)], writes=["mod"], dma_key="mo%d" % s)
            P.barrier()

        with ExitStack() as ph:
            psb = lambda n, s, dt: ph.enter_context(nc.sbuf_tensor(n, s, dt))
            scsh = psb("scsh", [128, 4, 2, KC], F32)
            for b in range(4):
                P.op("sp", lambda e, b=b: e.dma_start(out=scsh[:, b, 1, :], in_=mod[0, b, 0:D].rearrange("(k p) -> p k", p=128),
                                                      allow_slow_non_contiguous=True),
                     writes=[("scl", b, 1)], dma_key="c2")
                P.op("sp", lambda e, b=b: e.dma_start(out=scsh[:, b, 0, :], in_=mod[0, b, D:2 * D].rearrange("(k p) -> p k", p=128),
                                                      allow_slow_non_contiguous=True),
                     writes=[("scl", b, 0)], dma_key="c2")
            P.op("dve", lambda e: e.tensor_scalar(out=scsh[:, :, 0, :], in0=scsh[:, :, 0, :], scalar1=1.0, scalar2=None,
                                                  op0=ALU.add),
                 reads=[("scl", b, j) for b in range(4) for j in range(2)], writes=["scsh"])
            wsb = psb("wsb", [128, KC, EVEN_IN], BF16)
            wsw = psb("wsw", [128, KC, EVEN_IN], BF16)
            for k in range(KC):
                for (dst, src, nm) in ((wsb, w_in_even, "wsb"), (wsw, w_in_even_sw, "wsw")):
                    for h0 in (0, 1152):
                        P.op("pool", lambda e, dst=dst, src=src, k=k, h0=h0: e.dma_start(
                            out=dst[:, k, h0:h0 + 1152], in_=src[k * 128:(k + 1) * 128, h0:h0 + 1152]),
                            writes=[(nm, k, h0)], dma_key="w_" + nm)
            wsb_keys = [("wsb", k, h0) for k in range(KC) for h0 in (0, 1152)]
            wsw_keys = [("wsw", k, h0) for k in range(KC) for h0 in (0, 1152)]
            TB = 512
            xin = [psb("xin%d" % i, [128, D], F32) for i in range(2)]
            hT = [psb("hT%d" % i, [128, KC, TB], BF16) for i in range(2)]
            rc = [psb("rc%d" % i, [128, TB], F32) for i in range(2)]
            rs = [psb("rs%d" % i, [128, TB], F32) for i in range(2)]
            t1 = [psb("t1%d" % i, [128, TB], F32) for i in range(2)]
            t2 = [psb("t2%d" % i, [128, TB], F32) for i in range(2)]
            qo = [psb("qo%d" % i, [128, TB], BF16) for i in range(2)]
            vo = [psb("vo%d" % i, [128, 512], BF16) for i in range(2)]
            vbo = [psb("vbo%d" % i, [128, 130], BF16) for i in range(2)]
            for i in range(2):
                P.op("dve", lambda e, i=i: e.memset(vbo[i][:], 1.0), writes=[("vbo", i)])
            ROPED = set(range(0, 8)) | set(range(12, 17))
            nblk = nx = npt = nch = nv = 0
            for b, (off, ln, _S) in enumerate(cfg.segs):
                for t0 in range(0, ln, TB):
                    tb = min(TB, ln - t0)
                    hs = nblk % 2
                    nblk += 1
                    g0 = off + t0
                    for tt in range(tb // 128):
                        xs = nx % 2
                        nx += 1
                        P.op("sp", lambda e, xs=xs, r=g0 + tt * 128: e.dma_start(out=xin[xs][:], in_=x_loc[r:r + 128, :]),
                             writes=[("xin", xs)], dma_key="xin%d" % xs)
                        for half in range(2):
                            pp = 2 + npt % 2
                            npt += 1
                            for j in range(4):
                                k = half * 4 + j
                                P.op("pe", lambda e, pp=pp, xs=xs, k=k, j=j: e.transpose(
                                    out=BK[pp][:, j * 128:(j + 1) * 128], in_=xin[xs][:, k * 128:(k + 1) * 128],
                                    identity=idn[:]),
                                    reads=[("xin", xs), "idn"], writes=[bk(pp)])
                            for j in range(4):
                                k = half * 4 + j
                                P.op("act", lambda e, pp=pp, hs=hs, k=k, j=j, tt=tt, b=b: e.activation(
                                    out=hT[hs][:, k, tt * 128:(tt + 1) * 128], in_=BK[pp][:, j * 128:(j + 1) * 128],
                                    func=AF.Identity, scale=scsh[:, b, 0, k:k + 1], bias=scsh[:, b, 1, k:k + 1]),
                                    reads=[bk(pp), "scsh"], writes=[("hT", hs, k, tt)])
                    hT_keys = [("hT", hs, k, tt) for k in range(KC) for tt in range(tb // 128)]
                    rsl = nblk % 2
                    P.op("sp", lambda e, rsl=rsl, g0=g0, tb=tb: e.dma_start(out=rc[rsl][:, :tb], in_=ropeC[:, g0:g0 + tb]),
                         writes=[("rc", rsl)], dma_key="rc%d" % rsl)
                    P.op("sp", lambda e, rsl=rsl, g0=g0, tb=tb: e.dma_start(out=rs[rsl][:, :tb], in_=ropeS[:, g0:g0 + tb]),
                         writes=[("rs", rsl)], dma_key="rs%d" % rsl)
                    for ch in list(range(0, 8)) + list(range(12, 17)):
                        cs = nch % 2
                        nch += 1
                        pqb, pqsb = 4 + cs, 6 + cs
                        for k in range(KC):
                            P.op("pe", lambda e, pqb=pqb, hs=hs, k=k, ch=ch, tb=tb: e.matmul(
                                BK[pqb][:, :tb], lhsT=wsb[:, k, ch * 128:(ch + 1) * 128], rhs=hT[hs][:, k, :tb],
                                start=(k == 0), stop=(k == KC - 1)),
                                reads=hT_keys + wsb_keys, writes=[bk(pqb)])
                        if ch in ROPED:
                            for k in range(KC):
                                P.op("pe", lambda e, pqsb=pqsb, hs=hs, k=k, ch=ch, tb=tb: e.matmul(
                                    BK[pqsb][:, :tb], lhsT=wsw[:, k, ch * 128:(ch + 1) * 128], rhs=hT[hs][:, k, :tb],
                                    start=(k == 0), stop=(k == KC - 1)),
                                    reads=hT_keys + wsw_keys, writes=[bk(pqsb)])
                            P.op("dve", lambda e, cs=cs, pqb=pqb, rsl=rsl, tb=tb: e.tensor_tensor(
                                out=t1[cs][:, :tb], in0=BK[pqb][:, :tb], in1=rc[rsl][:, :tb], op=ALU.mult),
                                reads=[bk(pqb), ("rc", rsl)], writes=[("t1", cs)])
                            P.op("dve", lambda e, cs=cs, pqsb=pqsb, rsl=rsl, tb=tb: e.tensor_tensor(
                                out=t2[cs][:, :tb], in0=BK[pqsb][:, :tb], in1=rs[rsl][:, :tb], op=ALU.mult),
                                reads=[bk(pqsb), ("rs", rsl)], writes=[("t2", cs)])
                            P.op("pool", lambda e, cs=cs, tb=tb: e.tensor_tensor(
                                out=qo[cs][:, :tb], in0=t1[cs][:, :tb], in1=t2[cs][:, :tb], op=ALU.add),
                                reads=[("t1", cs), ("t2", cs)], writes=[("qo", cs)])
                        else:
                            P.op("act", lambda e, cs=cs, pqb=pqb, tb=tb: e.copy(out=qo[cs][:, :tb], in_=BK[pqb][:, :tb]),
                                 reads=[bk(pqb)], writes=[("qo", cs)])
                        if ch == 17:
                            continue
                        if ch == 16:
                            P.op("sp", lambda e, cs=cs, g0=g0, tb=tb: e.dma_start(
                                out=kbshare[g0:g0 + tb, :].rearrange("(j p) c -> p j c", p=128),
                                in_=qo[cs][:, :tb].rearrange("p (j c) -> p j c", c=128)),
                                reads=[("qo", cs)], writes=["kbshare"], dma_key="qo%d" % cs)
                            continue
                        if 4 <= ch < 8:
                            dst = kshare[(ch - 4) * 128:(ch - 3) * 128, g0:g0 + tb]
                            dk = "kshare"
                        else:
                            dst = projT[ch, :, g0:g0 + tb]
                            dk = "projT"
                        P.op("sp", lambda e, cs=cs, dst=dst, tb=tb: e.dma_start(out=dst, in_=qo[cs][:, :tb]),
                             reads=[("qo", cs)], writes=[dk], dma_key="qo%d" % cs)
                    for tt in range(tb // 128):
                        vs = nv % 2
                        nv += 1
                        for k in range(KC):
                            P.op("pe", lambda e, vs=vs, hs=hs, k=k, tt=tt: e.matmul(
                                BK[vs][:, :], lhsT=hT[hs][:, k, tt * 128:(tt + 1) * 128], rhs=wsb[:, k, 1024:1536],
                                start=(k == 0), stop=(k == KC - 1)),
                                reads=hT_keys + wsb_keys, writes=[bk(vs)])
                        P.op("act", lambda e, vs=vs: e.copy(out=vo[vs][:], in_=BK[vs][:, :]),
                             reads=[bk(vs)], writes=[("vo", vs)])
                        P.op("sp", lambda e, vs=vs, r=g0 + tt * 128: e.dma_start(out=vshare[r:r + 128, :], in_=vo[vs][:]),
                             reads=[("vo", vs)], writes=["vshare"], dma_key="vo%d" % vs)
                        vs = nv % 2
                        nv += 1
                        for k in range(KC):
                            P.op("pe", lambda e, vs=vs, hs=hs, k=k, tt=tt: e.matmul(
                                BK[vs][:, 0:128], lhsT=hT[hs][:, k, tt * 128:(tt + 1) * 128], rhs=wsb[:, k, 2176:2304],
                                start=(k == 0), stop=(k == KC - 1)),
                                reads=hT_keys + wsb_keys, writes=[bk(vs)])
                        P.op("act", lambda e, vs=vs: e.copy(out=vbo[vs][:, 0:64], in_=BK[vs][:, 0:64]),
                             reads=[bk(vs)], writes=[("vbo", vs)])
                        P.op("act", lambda e, vs=vs: e.copy(out=vbo[vs][:, 65:129], in_=BK[vs][:, 64:128]),
                             reads=[bk(vs)], writes=[("vbo", vs)])
                        P.op("sp", lambda e, vs=vs, r=g0 + tt * 128: e.dma_start(out=vbshare[r:r + 128, :], in_=vbo[vs][:]),
                             reads=[("vbo", vs)], writes=["vbshare"], dma_key="vbo%d" % vs)
            P.barrier()

        P.op("pool", lambda e: e.collective_compute("AllGather", ALU.bypass, replica_groups=[list(range(NCORES))],
                                                    ins=[kshare[:]], outs=[kgath[:]]),
             writes=["kgath"], dma_key="agk", inc=1)
        P.op("pool", lambda e: e.collective_compute("AllGather", ALU.bypass, replica_groups=[list(range(NCORES))],
                                                    ins=[vshare[:]], outs=[vgath[:]]),
             writes=["vgath"], dma_key="agv", inc=1)
        P.op("pool", lambda e: e.collective_compute("AllGather", ALU.bypass, replica_groups=[list(range(NCORES))],
                                                    ins=[kbshare[:]], outs=[kbgath[:]]),
             writes=["kbgath"], dma_key="agkb", inc=1)
        P.op("pool", lambda e: e.collective_compute("AllGather", ALU.bypass, replica_groups=[list(range(NCORES))],
                                                    ins=[vbshare[:]], outs=[vbgath[:]]),
             writes=["vbgath"], dma_key="agvb", inc=1)
        P.barrier()

        LAM_INIT = 0.2
        with ExitStack() as ph:
            psb = lambda n, s, dt: ph.enter_context(nc.sbuf_tensor(n, s, dt))
            lam = psb("lam", [1, 256], F32)
            lsc = psb("lsc", [1, 8], F32)
            neglam = psb("neglam", [128, 1], F32)
            subl = psb("subl", [128, 1], F32)
            P.op("sp", lambda e: e.dma_start(out=lam[:], in_=diff_lambda[:]), writes=["lam"], dma_key="c3")
            P.op("sp", lambda e: e.dma_start(out=subl[:], in_=diff_subln[:]), writes=["subl0"], dma_key="c4")
            P.op("dve", lambda e: e.tensor_scalar(out=subl[:], in0=subl[:], scalar1=1.0 - LAM_INIT, scalar2=None, op0=ALU.mult),
                 reads=["subl0"], writes=["subl"])
            prod = psb("prod", [1, 128], F32)
            P.op("dve", lambda e: e.tensor_tensor(out=prod[:, 0:64], in0=lam[:, 0:64], in1=lam[:, 64:128], op=ALU.mult),
                 reads=["lam"], writes=["prodA"])
            P.op("dve", lambda e: e.tensor_tensor(out=prod[:, 64:128], in0=lam[:, 128:192], in1=lam[:, 192:256], op=ALU.mult),
                 reads=["lam"], writes=["prodB"])
            P.op("dve", lambda e: e.tensor_reduce(out=lsc[:, 0:2], in_=prod[:].rearrange("p (a b) -> p a b", a=2),
                                                  op=ALU.add, axis=mybir.AxisListType.X),
                 reads=["prodA", "prodB"], writes=["lsc01"])
            P.op("act", lambda e: e.activation(out=lsc[:, 2:4], in_=lsc[:, 0:2], func=AF.Exp), reads=["lsc01"], writes=["lsc23"])
            P.op("dve", lambda e: e.tensor_tensor(out=lsc[:, 4:5], in0=lsc[:, 3:4], in1=lsc[:, 2:3], op=ALU.subtract),
                 reads=["lsc23"], writes=["lsc4"])
            P.op("dve", lambda e: e.tensor_scalar(out=lsc[:, 5:6], in0=lsc[:, 4:5], scalar1=-LAM_INIT, scalar2=None, op0=ALU.add),
                 reads=["lsc4"], writes=["lsc5"])
            P.op("pe", lambda e: e.matmul(BK[0][:, 0:1], lhsT=ones_f[0:1, :], rhs=lsc[:, 5:6], start=True, stop=True),
                 reads=["lsc5", "ones_f"], writes=[bk(0)])
            P.op("dve", lambda e: e.tensor_copy(out=neglam[:], in_=BK[0][:, 0:1]), reads=[bk(0)], writes=["neglam"])

            SMAX = max(sg[2] for sg in cfg.segs)
            LMAX = max(sg[1] for sg in cfg.segs)
            KT = psb("KT", [128, SMAX], BF16)
            VV = psb("VV", [128, SMAX // 128, 128], BF16)
            QT = psb("QT", [128, LMAX], BF16)
            QB = 512
            eb = [psb("eb%d" % i, [128, QB], BF16) for i in range(4)]
            rz = psb("rz", [128, QB], F32)
            o0 = psb("o0", [128, QB], F32)
            o1 = psb("o1", [128, QB], F32)
            osq = psb("osq", [128, QB], F32)
            rstd = psb("rstd", [128, QB], F32)
            ob = [psb("ob%d" % i, [128, QB], BF16) for i in range(2)]
            scale = 64 ** -0.5
            nst = 0
            nob = 0
            for b, (off, ln, S) in enumerate(cfg.segs):
                if stop_after == "A0":
                    break
                nchunk = S // 128
                for h in range(4):
                    for r in range(NCORES):
                        P.op("sp", lambda e, r=r, h=h, off=off, ln=ln: e.dma_start(
                            out=KT[:, r * ln:(r + 1) * ln], in_=kgath[r * 512 + h * 128:r * 512 + (h + 1) * 128, off:off + ln]),
                            reads=["kgath"], writes=[("KT", r)], dma_key="KT")
                        P.op("sp", lambda e, r=r, h=h, off=off, ln=ln: e.dma_start(
                            out=VV[:, r * (ln // 128):(r + 1) * (ln // 128), :],
                            in_=vgath[r * NT + off:r * NT + off + ln, h * 128:(h + 1) * 128].rearrange("(c p) d -> p c d", p=128)),
                            reads=["vgath"], writes=[("VV", r)], dma_key="VV")
                    P.op("sp", lambda e, h=h, off=off, ln=ln: e.dma_start(out=QT[:, :ln], in_=projT[h, :, off:off + ln]),
                         writes=["QT"], dma_key="QT")
                    kv_keys = [("KT", r) for r in range(NCORES)] + [("VV", r) for r in range(NCORES)]
                    for q0 in range(0, ln, QB):
                        qb = min(QB, ln - q0)
                        for c in range(nchunk):
                            si = 2 * (nst % 2)
                            nst += 1
                            for m in range(2):
                                P.op("pe", lambda e, si=si, m=m, c=c, q0=q0, qb=qb: e.matmul(
                                    BK[si + m][:, :qb], lhsT=KT[64 * m:64 * (m + 1), c * 128:(c + 1) * 128],
                                    rhs=QT[64 * m:64 * (m + 1), q0:q0 + qb], start=True, stop=True),
                                    reads=kv_keys + ["QT"], writes=[bk(si + m)])
                            for m in range(2):
                                P.op("act", lambda e, si=si, m=m, qb=qb: e.activation(
                                    out=eb[si + m][:, :qb], in_=BK[si + m][:, :qb], func=AF.Exp, scale=scale),
                                    reads=[bk(si + m)], writes=[("eb", si + m)])
                            for m in range(2):
                                P.op("pe", lambda e, si=si, m=m, c=c, qb=qb, nchunk=nchunk: e.matmul(
                                    BK[4 + m][:, :qb], lhsT=VV[:, c, :], rhs=eb[si + m][:, :qb],
                                    start=(c == 0), stop=(c == nchunk - 1)),
                                    reads=kv_keys + [("eb", si + m)], writes=[bk(4 + m)])
                                P.op("pe", lambda e, si=si, m=m, c=c, qb=qb, nchunk=nchunk: e.matmul(
                                    BK[6 + m][:, :qb], lhsT=ones_bf[:, :], rhs=eb[si + m][:, :qb],
                                    start=(c == 0), stop=(c == nchunk - 1)),
                                    reads=["ones_bf", ("eb", si + m)], writes=[bk(6 + m)])
                        P.op("dve", lambda e, qb=qb: e.reciprocal(out=rz[:, :qb], in_=BK[6][:, :qb]), reads=[bk(6)], writes=["rz"])
                        P.op("dve", lambda e, qb=qb: e.tensor_tensor(out=o0[:, :qb], in0=BK[4][:, :qb], in1=rz[:, :qb], op=ALU.mult),
                             reads=[bk(4), "rz"], writes=["o0"])
                        P.op("dve", lambda e, qb=qb: e.reciprocal(out=rz[:, :qb], in_=BK[7][:, :qb]), reads=[bk(7)], writes=["rz"])
                        P.op("dve", lambda e, qb=qb: e.tensor_tensor(out=o1[:, :qb], in0=BK[5][:, :qb], in1=rz[:, :qb], op=ALU.mult),
                             reads=[bk(5), "rz"], writes=["o1"])
                        P.op("dve", lambda e, qb=qb: e.scalar_tensor_tensor(
                            out=o0[:, :qb], in0=o1[:, :qb], scalar=neglam[:, 0:1], in1=o0[:, :qb], op0=ALU.mult, op1=ALU.add),
                            reads=["o0", "o1", "neglam"], writes=["o0"])
                        P.op("pool", lambda e, qb=qb: e.tensor_tensor(out=osq[:, :qb], in0=o0[:, :qb], in1=o0[:, :qb], op=ALU.mult),
                             reads=["o0"], writes=["osq"])
                        P.op("pe", lambda e, qb=qb: e.matmul(BK[0][:, :qb], lhsT=ones_f[:, :], rhs=osq[:, :qb], start=True, stop=True),
                             reads=["ones_f", "osq"], writes=[bk(0)])
                        P.op("dve", lambda e, qb=qb: e.tensor_scalar(out=rstd[:, :qb], in0=BK[0][:, :qb], scalar1=1.0 / 128, scalar2=1e-6,
                                                                     op0=ALU.mult, op1=ALU.add),
                             reads=[bk(0)], writes=["rstd"])
                        P.op("act", lambda e, qb=qb: e.activation(out=rstd[:, :qb], in_=rstd[:, :qb], func=AF.Ln),
                             reads=["rstd"], writes=["rstd"])
                        P.op("act", lambda e, qb=qb: e.activation(out=rstd[:, :qb], in_=rstd[:, :qb], func=AF.Exp, scale=-0.5),
                             reads=["rstd"], writes=["rstd"])
                        P.op("dve", lambda e, qb=qb: e.tensor_tensor(out=o1[:, :qb], in0=o0[:, :qb], in1=rstd[:, :qb], op=ALU.mult),
                             reads=["o0", "rstd"], writes=["o1"])
                        os_ = nob % 2
                        nob += 1
                        P.op("act", lambda e, qb=qb, os_=os_: e.activation(out=ob[os_][:, :qb], in_=o1[:, :qb], func=AF.Identity,
                                                                          scale=subl[:, 0:1]),
                             reads=["o1", "subl"], writes=[("ob", os_)])
                        P.op("sp", lambda e, qb=qb, os_=os_, h=h, g=off + q0: e.dma_start(out=attnT[h, :, g:g + qb], in_=ob[os_][:, :qb]),
                             reads=[("ob", os_)], writes=["attnT"], dma_key="ob%d" % os_)
            P.barrier()

        with ExitStack() as ph:
            psb = lambda n, s, dt: ph.enter_context(nc.sbuf_tensor(n, s, dt))
            LMAX = max(sg[1] for sg in cfg.segs)
            NBM = LMAX // 128
            wm = psb("wm", [128, 2, 512], BF16)
            for j in range(2):
                P.op("sp", lambda e, j=j: e.dma_start(out=wm[:, j, :], in_=wmask[j]), writes=[("wm", j)], dma_key="c5")
            sk = psb("sk", [128, 8], F32)
            esr = psb("esr", [128, 2, 512], BF16)
            oh = psb("oh", [128, 65], BF16)
            P.op("sp", lambda e: e.dma_start(out=sk[64:65, :], in_=swa_sink[:]), writes=["sk0"], dma_key="c6")
            P.op("act", lambda e: e.activation(out=sk[64:65, :], in_=sk[64:65, :], func=AF.Exp), reads=["sk0"], writes=["sk"])
            P.op("dve", lambda e: e.memset(esr[64:65, :, :], 1.0), writes=["esr0"])
            for hq in range(8):
                P.op("dve", lambda e, hq=hq: e.tensor_scalar(
                    out=esr[64:65, hq // 4, (hq % 4) * 128:(hq % 4 + 1) * 128], in0=esr[64:65, hq // 4, (hq % 4) * 128:(hq % 4 + 1) * 128],
                    scalar1=sk[64:65, hq:hq + 1], scalar2=None, op0=ALU.mult),
                    reads=["sk", "esr0"] + ([("esrh", hq - 1)] if hq else []), writes=[("esrh", hq)])
            P.op("dve", lambda e: e.memset(oh[64:65, :], 0.0), writes=["oh0"])
            P.op("dve", lambda e: e.memset(oh[64:65, 64:65], 1.0), reads=["oh0"], writes=["oh"])
            idt = psb("idt", [128, 8], I32)
            for b in range(4):
                for sd in range(2):
                    P.op("sp", lambda e, b=b, sd=sd: e.dma_start(out=idt[:, b * 2 + sd:b * 2 + sd + 1], in_=idx_w[b, sd]),
                         writes=[("idt", b, sd)], dma_key="c7")
            KTw = psb("KTw", [128, (NBM + 2) * 128], BF16)
            Vw = psb("Vw", [128, NBM + 2, 130], BF16)
            QTa = psb("QTa", [128, 4, LMAX], BF16)
            ew = [psb("ew%d" % i, [128, 512], BF16) for i in range(3)]
            rzw = psb("rzw", [128, 512], F32)
            obw = [psb("obw%d" % i, [64, 512], BF16) for i in range(2)]
            scale = 64 ** -0.5
            nit = 0
            for b, (off, ln, S) in enumerate(cfg.segs):
                if stop_after in ("A0", "B0"):
                    break
                nb = ln // 128
                P.op("dve", lambda e: e.memset(KTw[:, 0:128], 0.0), reads=["KTw"], writes=[("KTh", 0)])
                P.op("dve", lambda e, nb=nb: e.memset(KTw[:, (nb + 1) * 128:(nb + 2) * 128], 0.0), reads=["KTw"], writes=[("KTh", 1)])
                P.op("dve", lambda e: e.memset(Vw[:, 0, :], 0.0), reads=["KTw"], writes=[("Vh", 0)])
                P.op("dve", lambda e, nb=nb: e.memset(Vw[:, nb + 1, :], 0.0), reads=["KTw"], writes=[("Vh", 1)])
                P.op("sp", lambda e, off=off, ln=ln: e.dma_start(
                    out=KTw[:, 128:128 + ln].rearrange("p (j c) -> p j c", c=128),
                    in_=kbshare[off:off + ln, :].rearrange("(j p) c -> p j c", p=128)),
                    reads=["KTw"], writes=["KTl"], dma_key="KTw")
                P.op("sp", lambda e, off=off, ln=ln, nb=nb: e.dma_start(
                    out=Vw[:, 1:1 + nb, :], in_=vbshare[off:off + ln, :].rearrange("(j p) c -> p j c", p=128)),
                    reads=["KTw"], writes=["Vl"], dma_key="Vw")
                for sd in range(2):
                    kcol = 0 if sd == 0 else (nb + 1) * 128
                    vblk = 0 if sd == 0 else nb + 1
                    P.op("pool", lambda e, b=b, sd=sd, kcol=kcol: e.indirect_dma_start(
                        out=KTw[:, kcol:kcol + 128], out_offset=None, in_=kbgath[:, :],
                        in_offset=bass.IndirectOffsetOnAxis(ap=idt[:, b * 2 + sd:b * 2 + sd + 1], axis=0),
                        bounds_check=NCORES * NT - 1, oob_is_err=False),
                        reads=[("idt", b, sd), "kbgath"], writes=[("KTh", sd)], dma_key="KTh%d" % sd)
                    P.op("pool", lambda e, b=b, sd=sd, vblk=vblk: e.indirect_dma_start(
                        out=Vw[:, vblk, :], out_offset=None, in_=vbgath[:, :],
                        in_offset=bass.IndirectOffsetOnAxis(ap=idt[:, b * 2 + sd:b * 2 + sd + 1], axis=0),
                        bounds_check=NCORES * NT - 1, oob_is_err=False),
                        reads=[("idt", b, sd), "vbgath"], writes=[("Vh", sd)], dma_key="Vh%d" % sd)
                for c4 in range(4):
                    P.op("sp", lambda e, c4=c4, off=off, ln=ln: e.dma_start(out=QTa[:, c4, :ln], in_=projT[12 + c4, :, off:off + ln]),
                         reads=["KTw"], writes=[("QTa", c4)], dma_key="QTa")
                kvk = ["KTl", "Vl", ("KTh", 0), ("KTh", 1), ("Vh", 0), ("Vh", 1)] + [("QTa", c4) for c4 in range(4)]
                for n in range(nb):
                    for g in range(2):
                        pO = 4 + (nit % 2)
                        os_ = nit % 2
                        nit += 1
                        for kc in range(3):
                            P.op("pe", lambda e, kc=kc, n=n, g=g: e.matmul(
                                BK[kc][:, :], lhsT=KTw[g * 64:(g + 1) * 64, (n + kc) * 128:(n + kc + 1) * 128],
                                rhs=QTa[g * 64:(g + 1) * 64, :, n * 128:(n + 1) * 128], start=True, stop=True),
                                reads=kvk, writes=[bk(kc)])
                            P.op("act", lambda e, kc=kc: e.activation(out=ew[kc][:], in_=BK[kc][:, :], func=AF.Exp, scale=scale),
                                 reads=[bk(kc)], writes=[("ew", kc)])
                            if kc != 1:
                                P.op("dve", lambda e, kc=kc: e.tensor_tensor(out=ew[kc][:], in0=ew[kc][:], in1=wm[:, kc // 2, :], op=ALU.mult),
                                     reads=[("ew", kc), ("wm", kc // 2)], writes=[("ew", kc)])
                        for kc in range(3):
                            P.op("pe", lambda e, kc=kc, n=n, g=g, pO=pO: e.matmul(
                                BK[pO][0:65, :], lhsT=Vw[:, n + kc, g * 65:(g + 1) * 65], rhs=ew[kc][:],
                                start=(kc == 0), stop=False),
                                reads=kvk + [("ew", kc)], writes=[bk(pO)])
                        P.op("pe", lambda e, g=g, pO=pO: e.matmul(BK[pO][0:65, :], lhsT=oh[64:65, :], rhs=esr[64:65, g, :],
                                                                 start=False, stop=True),
                             reads=["oh"] + [("esrh", 7)], writes=[bk(pO)])
                        P.op("dve", lambda e, pO=pO: e.reciprocal(out=rzw[64:65, :], in_=BK[pO][64:65, :]), reads=[bk(pO)], writes=["rzw"])
                        P.op("pe", lambda e: e.matmul(BK[3][0:64, :], lhsT=ones_f[64:65, 0:64], rhs=rzw[64:65, :], start=True, stop=True),
                             reads=["rzw", "ones_f"], writes=[bk(3)])
                        P.op("dve", lambda e: e.tensor_copy(out=rzw[0:64, :], in_=BK[3][0:64, :]), reads=[bk(3)], writes=["rzb"])
                        P.op("dve", lambda e, pO=pO, os_=os_: e.tensor_tensor(out=obw[os_][:, :], in0=BK[pO][0:64, :], in1=rzw[0:64, :], op=ALU.mult),
                             reads=[bk(pO), "rzb"], writes=[("obw", os_)])
                        for j in range(4):
                            hq = g * 4 + j
                            P.op("sp", lambda e, os_=os_, j=j, hq=hq, t=off + n * 128: e.dma_start(
                                out=attnT[4 + hq // 2, (hq % 2) * 64:(hq % 2) * 64 + 64, t:t + 128], in_=obw[os_][:, j * 128:(j + 1) * 128]),
                                reads=[("obw", os_)], writes=["attnT"], dma_key="obw%d" % os_)
                P.op("dve", lambda e: e.memset(rzw[0:1, 0:1], 0.0), reads=kvk, writes=["KTw"])
            P.barrier()

        fwx = []
        full_done = False
        if stop_after not in ("A0", "B0", "B0w"):
            fwx += resid_ln_phase(nc, P, BK, cfg, "c0_", 0, 0, mod, ln_g, ln_b, x_loc, xa, "proj", attnT, w_out_even, 2 * D)
        if stop_after not in ("A0", "B0", "B0w", "C0"):
            moe_phase(nc, P, BK, cfg, "e0_", 0, mod, idn, ones_f, msel_in, router_w, wg_f, wu_f, wd_f,
                      xa, h2T_d, moe_acc, affshare, affgath)
            fwx += resid_ln_phase(nc, P, BK, cfg, "f0_", 0, 1, mod, ln_g, ln_b, xa, xb, "acc", moe_acc, None, 5 * D)

        if stop_after not in ("A0", "B0", "B0w", "C0", "E0"):
            l1_proj_phase(nc, P, BK, cfg, mod, idn, ones_f, xb, w_in_odd, w_kr_sw, mla_q_norm, mla_kv_norm, mla_w_uq, mla_w_uq_sw,
                          mla_w_ukv, ropeC96, ropeS96, mlaQ, mlaKsh, mlaVsh, naQ, naKsh, naVsh)
            ag_list = [(mlaKsh, mlaKg, "mk"), (mlaVsh, mlaVg, "mv")] + [(naKsh[c], naKg[c], "nk%d" % c) for c in range(4)] + \
                      [(naVsh[c], naVg[c], "nv%d" % c) for c in range(4)]
            for (src, dst, nm) in ag_list:
                P.op("pool", lambda e, src=src, dst=dst: e.collective_compute("AllGather", ALU.bypass, replica_groups=[list(range(NCORES))],
                                                                             ins=[src[:]], outs=[dst[:]]),
                     writes=[nm], dma_key="ag1", inc=1)
            P.barrier()
            fwx += mla_attn_phase(nc, P, BK, cfg, ones_f, mlaQ, mlaKg, mlaVg, attnT1)
            if stop_after != "MLA":
                fwx += na_attn_phase(nc, P, BK, cfg, ones_f, naQ, naKsh, naVsh, naKg, naVg, na_bt, na_rmask, idx_n, attnT1)
            if stop_after not in ("MLA", "NA"):
                fwx += resid_ln_phase(nc, P, BK, cfg, "c1_", 1, 0, mod, ln_g, ln_b, xb, xa, "proj", attnT1, w_out_odd, 2 * D)
                moe_phase(nc, P, BK, cfg, "e1_", 1, mod, idn, ones_f, msel_in, router_w, wg_f, wu_f, wd_f,
                          xa, h2T_d, moe_acc, affshare, affgath)
                fwx += resid_ln_phase(nc, P, BK, cfg, "f1_", 1, 1, mod, ln_g, ln_b, xa, y_loc, "acc", moe_acc, None, 5 * D)
                full_done = True

        if not full_done:
            zt = sb("zt", [128, D], F32)
            for i in range(NT // 128):
                P.op("sp", lambda e, i=i: e.dma_start(out=zt[:], in_=x_loc[i * 128:(i + 1) * 128, :]),
                     writes=["zt"], dma_key="zt_l")
                P.op("sp", lambda e, i=i: e.dma_start(out=y_loc[i * 128:(i + 1) * 128, :], in_=zt[:]),
                     reads=["zt"], writes=["y"], dma_key="zt_s")
        fw = ["zt_s", "ob0", "ob1", "mo0", "mo1", "obw0", "obw1"] + fwx
        P.emit(final_waits=fw)
    return nc


def rope_tables_fm(pos):
    inv = 1.0 / (10000.0 ** (np.arange(0, 64, 2, dtype=np.float32) / 64))
    ang = pos.astype(np.float32)[None, :] * inv[:, None]
    c, s = np.cos(ang), np.sin(ang)
    C = np.concatenate([c, c, c, c], 0).astype(np.float32)
    S = np.concatenate([-s, s, -s, s], 0).astype(np.float32)
    return C, S


def swap_halves_cols(w):
    n = w.shape[-1]
    idx = np.arange(n).reshape(n // 64, 2, 32)[:, ::-1, :].reshape(n)
    return np.ascontiguousarray(w[..., idx])


def permute_qb_cols(w):
    w = w.copy()
    qb = w[:, 1536:2048].reshape(w.shape[0], 8, 64)
    new = np.stack([np.concatenate([qb[:, c], qb[:, 4 + c]], -1) for c in range(4)], 1)
    w[:, 1536:2048] = new.reshape(w.shape[0], 512)
    return w


def window_consts(cfg, core):
    import ml_dtypes
    a = np.arange(128)[:, None]
    q = np.arange(128)[None, :]
    mL = (q <= a).astype(np.float32)
    mR = (a <= q).astype(np.float32)
    wmask = np.stack([np.tile(mL, (1, 4)), np.tile(mR, (1, 4))], 0).astype(ml_dtypes.bfloat16)
    OOB = 1 << 30
    idx = np.full((4, 2, 128, 1), OOB, np.int32)
    for b, (off, ln, S) in enumerate(cfg.segs):
        if core > 0:
            idx[b, 0, :, 0] = (core - 1) * cfg.nt + off + ln - 128 + np.arange(128)
        if core < NCORES - 1:
            idx[b, 1, :, 0] = (core + 1) * cfg.nt + off + np.arange(128)
    return wmask, idx


def rope96_tables(pos):
    inv = 1.0 / (10000.0 ** (np.arange(0, 32, 2, dtype=np.float32) / 32))
    ang = pos.astype(np.float32)[None, :] * inv[:, None]
    c, s_ = np.cos(ang), np.sin(ang)
    n = pos.shape[0]
    C = np.concatenate([np.ones((64, n), np.float32), c, c], 0).astype(np.float32)
    S = np.concatenate([np.zeros((64, n), np.float32), -s_, s_], 0).astype(np.float32)
    return C, S


def l1_weight_layouts(inp):
    w_in = np.ascontiguousarray(inp["w_in_odd"][0])
    kr = w_in[:, 384:416]
    w_kr_sw = np.ascontiguousarray(np.concatenate([kr[:, 16:], kr[:, :16]], 1))
    wuq = np.ascontiguousarray(inp["mla_w_uq"][0])
    sw = wuq.reshape(256, 8, 96).copy()
    sw[:, :, 64:96] = np.concatenate([sw[:, :, 80:96], sw[:, :, 64:80]], -1)
    wukv = inp["mla_w_ukv"][0].reshape(128, 8, 128)
    wukv_p = np.ascontiguousarray(np.concatenate([wukv[:, :, :64].reshape(128, 512), wukv[:, :, 64:].reshape(128, 512)], 1))
    return w_in, w_kr_sw, wuq, np.ascontiguousarray(sw.reshape(256, 768)), wukv_p


NEG = -30000.0


def na_bias_table(rpb):
    import ml_dtypes
    half = np.arange(2)[:, None, None, None, None]
    kcol = np.arange(64)[None, :, None, None, None]
    kc = np.arange(9)[None, None, :, None, None]
    qq = np.arange(2)[None, None, None, :, None]
    qcol = np.arange(64)[None, None, None, None, :]
    dr = 2 * kc + half - 1 - qq + 0 * kcol + 0 * qcol
    qcs = np.clip(qcol - 8, 0, 48)
    valid = (dr >= 0) & (dr <= 14) & (kcol >= qcs) & (kcol < qcs + 16)
    dc = np.clip(kcol - qcol + 15, 0, 30) + 0 * dr
    drc = np.clip(dr, 0, 14)
    out = np.empty((128, 8, 9, 128), np.float32)
    for h in range(8):
        v = np.where(valid, rpb[h][drc, dc], NEG)
        out[:, h] = v.reshape(128, 9, 128)
    return np.ascontiguousarray(out.reshape(128, 8 * 9 * 128)).astype(ml_dtypes.bfloat16)


def na_consts(cfg, core):
    NT = cfg.nt
    rmask = np.zeros((128, NT // 128, 9, 2), np.float32)
    idx = np.zeros((4, 8, 128), np.int32)
    p = np.arange(128)
    pair0 = 0
    for b, (off, ln, S) in enumerate(cfg.segs):
        rows = S // 64
        nr = ln // 64
        R0 = core * nr
        for m in range(ln // 128):
            for kc in range(9):
                for half in range(2):
                    kr = R0 + 2 * m - 8 + 2 * kc + half
                    for qq in range(2):
                        qr = R0 + 2 * m + qq
                        st = min(max(qr - 4, 0), rows - 8)
                        ok = (0 <= kr < rows) and (st <= kr < st + 8)
                        rmask[half * 64:(half + 1) * 64, pair0 + m, kc, qq] = 0.0 if ok else NEG
        pair0 += ln // 128
        for hb in range(8):
            g = core * ln - 512 + hb * 128 if hb < 4 else (core + 1) * ln + (hb - 4) * 128
            if g < 0 or g >= S:
                continue
            rank, lt = g // ln, g % ln
            idx[b, hb, :] = rank * NT + off + lt + p
    return np.ascontiguousarray(rmask.reshape(128, -1)), np.ascontiguousarray(idx.reshape(32, 128).T)


def make_in_maps(cfg, inp):
    maps = []
    na_bt_host = na_bias_table(np.asarray(inp["na_rpb"][0]))
    w_in1, w_kr_sw, wuq, wuq_sw, wukv_p = l1_weight_layouts(inp)
    w = permute_qb_cols(np.ascontiguousarray(inp["w_in_even"][0]))
    wsw = swap_halves_cols(w)
    c_all = np.concatenate([inp["c_prompt"], inp["c_sample"]], 0)
    for i in range(NCORES):
        xs = [inp["x_prompt"][0, i * cfg.lp:(i + 1) * cfg.lp], inp["x_prompt"][1, i * cfg.lp:(i + 1) * cfg.lp],
              inp["x_sample"][0, i * cfg.ls:(i + 1) * cfg.ls], inp["x_sample"][1, i * cfg.ls:(i + 1) * cfg.ls]]
        pos = np.concatenate([np.arange(i * cfg.lp, (i + 1) * cfg.lp)] * 2 + [np.arange(i * cfg.ls, (i + 1) * cfg.ls)] * 2)
        C, S = rope_tables_fm(pos)
        maps.append({
            "x_loc": np.ascontiguousarray(np.concatenate(xs, 0)), "c_all": c_all,
            "ident": np.eye(128, dtype=np.float32), "ropeC": C, "ropeS": S,
            "w_in_even": w, "w_in_even_sw": wsw,
            "ada_w": inp["ada_w"], "ada_b": inp["ada_b"],
            "diff_lambda": np.ascontiguousarray(inp["diff_lambda"][0].reshape(1, 256)),
            "diff_subln": np.ascontiguousarray(inp["diff_subln"][0].reshape(128, 1)),
            "swa_sink": np.ascontiguousarray(inp["swa_sink"][0].reshape(1, 8)),
            "wmask": window_consts(cfg, i)[0], "idx_w": window_consts(cfg, i)[1],
            "w_out_even": np.ascontiguousarray(inp["w_out_even"][0]),
            "ln_g": inp["ln_g"], "ln_b": inp["ln_b"], "router_w": inp["router_w"],
            "wg_sh": np.ascontiguousarray(inp["exp_w_gate"][:, 2 * i:2 * i + 2].reshape(2, 2 * D, 2816)),
            "wu_sh": np.ascontiguousarray(inp["exp_w_up"][:, 2 * i:2 * i + 2].reshape(2, 2 * D, 2816)),
            "wd_sh": np.ascontiguousarray(inp["exp_w_down"][:, 2 * i:2 * i + 2].reshape(2, 2 * 2816, D)),
            "msel": (np.arange(128)[:, None] % 16 == np.arange(128)[None, :] % 16).astype(np.float32),
            "w_in_odd": w_in1, "w_kr_sw": w_kr_sw, "mla_w_uq": wuq, "mla_w_uq_sw": wuq_sw, "mla_w_ukv": wukv_p,
            "mla_q_norm": np.ascontiguousarray(inp["mla_q_norm"][0].reshape(2, 128).T),
            "mla_kv_norm": np.ascontiguousarray(inp["mla_kv_norm"][0].reshape(128, 1)),
            "ropeC96": rope96_tables(pos)[0], "ropeS96": rope96_tables(pos)[1],
            "w_out_odd": np.ascontiguousarray(inp["w_out_odd"][0]),
            "na_bt": na_bt_host, "na_rmask": na_consts(cfg, i)[0], "idx_n": na_consts(cfg, i)[1],
        })
    return maps


def kernel(**inp):
    cfg = Cfg(inp["x_prompt"].shape[1], inp["x_sample"].shape[1])
    nc = build(cfg)
    res = run_bass_kernel_spmd(nc, make_in_maps(cfg, inp), core_ids=list(range(NCORES)))
    yp = np.zeros_like(inp["x_prompt"])
    ys = np.zeros_like(inp["x_sample"])
    for i in range(NCORES):
        y = res.results[i]["y_loc"]
        yp[0, i * cfg.lp:(i + 1) * cfg.lp] = y[0:cfg.lp]
        yp[1, i * cfg.lp:(i + 1) * cfg.lp] = y[cfg.lp:2 * cfg.lp]
        ys[0, i * cfg.ls:(i + 1) * cfg.ls] = y[2 * cfg.lp:2 * cfg.lp + cfg.ls]
        ys[1, i * cfg.ls:(i + 1) * cfg.ls] = y[2 * cfg.lp + cfg.ls:]
    return yp, ys
```
